# Optimizing a Trainium2 kernel written in Bass

```python
import math
import jax
import jax.numpy as jnp
from jax import lax
import numpy as np


D_MODEL = 2048
BATCH = 8
SEQ = 2048
DEPTH = 2

HEAD_DIM = 128
MIX_WIDTH = D_MODEL
A_WIDTH = MIX_WIDTH // 2
A_GROUPS = A_WIDTH // HEAD_DIM
B_HEADS = (MIX_WIDTH - A_WIDTH) // HEAD_DIM
B_WIDTH = B_HEADS * HEAD_DIM
C_WIDTH = MIX_WIDTH // 2
D_HEADS = (MIX_WIDTH - C_WIDTH) // HEAD_DIM
D_WIDTH = D_HEADS * HEAD_DIM
IN_AB = 2 * A_WIDTH + 3 * B_WIDTH
IN_CD = 3 * C_WIDTH + 3 * D_WIDTH
CHUNK = 128
GRID_W = 64
NA_ROWS = 8
NA_COLS = 16
NA_QBLK = 16
NA_BAND = 32
CONV_W = 3
DIL_PATTERNS = ((128, 1), (512, 4), (2048, 16))
DIL_BLOCK = 64
N_EXPERTS = 16
EC_CAPACITY_FACTOR = 2
D_EXPERT = 2048
N_EVEN = (DEPTH + 1) // 2
N_ODD = DEPTH // 2
RMS_EPS = 1e-6
LN_EPS = 1e-5
NEG_INF = -1e30

kernel_name = "hybrid_sgu_natten_shortconv_dilated_ec_moe"


def rms_norm(x, g):
    xf = x.astype(jnp.float32)
    y = xf * lax.rsqrt(jnp.mean(xf * xf, axis=-1, keepdims=True) + RMS_EPS)
    return (y * g.astype(jnp.float32)).astype(x.dtype)


def alibi_slopes(n):
    return np.array([2.0 ** (-8.0 * (h + 1) / n) for h in range(n)], dtype=np.float32)


def spatial_gating_unit(u_raw, v_raw, ln_g, w_s, b_s):
    bsz, seq, _ = u_raw.shape
    u = jax.nn.gelu(u_raw)
    vf = jax.nn.gelu(v_raw).astype(jnp.float32)
    mu = jnp.mean(vf, axis=-1, keepdims=True)
    var = jnp.mean(jnp.square(vf - mu), axis=-1, keepdims=True)
    v = ((vf - mu) * lax.rsqrt(var + LN_EPS) * ln_g.astype(jnp.float32)).astype(u_raw.dtype)
    v = v.reshape(bsz, seq // CHUNK, CHUNK, A_GROUPS, HEAD_DIM)
    mixed = jnp.einsum('gts,bnsgc->bntgc', w_s, v) + b_s.T[None, None, :, :, None]
    return u * mixed.reshape(bsz, seq, A_WIDTH)


def neighbourhood_attention(q, k, v, rpb):
    bsz, seq, nh, dh = q.shape
    rows = seq // GRID_W
    kh = min(NA_ROWS, rows)
    scale = dh ** -0.5
    qg = q.reshape(bsz, rows, GRID_W, nh, dh)
    kg = k.reshape(bsz, rows, GRID_W, nh, dh)
    vg = v.reshape(bsz, rows, GRID_W, nh, dh)
    row_start = jnp.asarray(np.clip(np.arange(rows) - kh // 2, 0, rows - kh), jnp.int32)
    col_start = np.clip(np.arange(GRID_W) - NA_COLS // 2, 0, GRID_W - NA_COLS)
    n_cb = GRID_W // NA_QBLK
    band0 = np.clip(np.arange(n_cb) * NA_QBLK - NA_COLS // 2, 0, GRID_W - NA_BAND)
    key_cols = band0[:, None] + np.arange(NA_BAND)
    q_cols = np.arange(n_cb)[:, None] * NA_QBLK + np.arange(NA_QBLK)
    kc = key_cols[:, None, :]
    qc = q_cols[:, :, None]
    col_valid = jnp.asarray((kc >= col_start[qc]) & (kc < col_start[qc] + NA_COLS))
    dc_idx = np.clip(kc - qc + NA_COLS - 1, 0, 2 * NA_COLS - 2)
    rpb_cols = rpb[:, :, dc_idx]

    def row_block(r):
        rs = row_start[r]
        k_rows = lax.dynamic_slice_in_dim(kg, rs, kh, axis=1)
        v_rows = lax.dynamic_slice_in_dim(vg, rs, kh, axis=1)
        k_band = k_rows[:, :, key_cols]
        v_band = v_rows[:, :, key_cols]
        q_row = lax.dynamic_index_in_dim(qg, r, axis=1, keepdims=False).reshape(bsz, n_cb, NA_QBLK, nh, dh)
        s = jnp.einsum('bcihd,bacjhd->bhciaj', q_row, k_band).astype(jnp.float32) * scale
        bias = rpb_cols[:, rs + jnp.arange(kh) - r + NA_ROWS - 1]
        s = s + bias.transpose(0, 2, 3, 1, 4)[None].astype(jnp.float32)
        s = jnp.where(col_valid[None, None, :, :, None, :], s, NEG_INF)
        p = jax.nn.softmax(s, axis=(-2, -1))
        o = jnp.einsum('bhciaj,bacjhd->bcihd', p.astype(v.dtype), v_band)
        return o.reshape(bsz, GRID_W, nh, dh)

    out = lax.map(row_block, jnp.arange(rows, dtype=jnp.int32))
    return out.transpose(1, 0, 2, 3, 4).reshape(bsz, seq, nh * dh)


def dilated_branch(q, k, v, dil, radius, slopes):
    bsz, seq, nh, dh = q.shape
    length = seq // dil
    blk = DIL_BLOCK
    nb = -(-length // blk)
    lp = nb * blk
    n = bsz * dil
    scale = dh ** -0.5

    def to_sub(t):
        return t.reshape(bsz, length, dil, nh, dh).transpose(0, 2, 1, 3, 4).reshape(n, length, nh, dh)

    def band(t):
        tp = jnp.pad(t, ((0, 0), (blk, lp - length + blk), (0, 0), (0, 0))).reshape(n, nb + 2, blk, nh, dh)
        return jnp.concatenate([tp[:, :-2], tp[:, 1:-1], tp[:, 2:]], axis=2)

    qs = jnp.pad(to_sub(q), ((0, 0), (0, lp - length), (0, 0), (0, 0))).reshape(n, nb, blk, nh, dh)
    kb = band(to_sub(k))
    vb = band(to_sub(v))
    s = jnp.einsum('nqihd,nqjhd->nqhij', qs, kb).astype(jnp.float32) * scale
    delta = np.arange(3 * blk)[None, :] - blk - np.arange(blk)[:, None]
    tk = (np.arange(nb)[:, None] - 1) * blk + np.arange(3 * blk)[None, :]
    valid = (np.abs(delta) <= radius)[None] & ((tk >= 0) & (tk < length))[:, None, :]
    penalty = (slopes[:, None, None] * (np.abs(delta) * dil)[None]).astype(np.float32)
    s = s - jnp.asarray(penalty)[None, None]
    s = jnp.where(jnp.asarray(valid)[None, :, None], s, NEG_INF)
    m = jnp.max(s, axis=-1, keepdims=True)
    e = jnp.exp(s - m)
    l = jnp.sum(e, axis=-1, keepdims=True)
    o = jnp.einsum('nqhij,nqjhd->nqihd', (e / l).astype(v.dtype), vb).reshape(n, lp, nh, dh)[:, :length]
    lse = (m + jnp.log(l))[..., 0].transpose(0, 1, 3, 2).reshape(n, lp, nh)[:, :length]
    o = o.reshape(bsz, dil, length, nh, dh).transpose(0, 2, 1, 3, 4).reshape(bsz, seq, nh, dh)
    lse = lse.reshape(bsz, dil, length, nh).transpose(0, 2, 1, 3).reshape(bsz, seq, nh)
    return o, lse


def dilated_attention(q, k, v):
    bsz, seq, nh, dh = q.shape
    slopes = alibi_slopes(nh)
    outs, lses = [], []
    for window, dil in DIL_PATTERNS:
        o, lse = dilated_branch(q, k, v, dil, window // (2 * dil), slopes)
        outs.append(o)
        lses.append(lse)
    alpha = jax.nn.softmax(jnp.stack(lses), axis=0)
    out = jnp.einsum('pbsh,pbshd->bshd', alpha.astype(q.dtype), jnp.stack(outs))
    return out.reshape(bsz, seq, nh * dh)


def gated_short_conv(b_gate, c_gate, xin, taps):
    z = c_gate * xin
    y = lax.conv_general_dilated(z, taps[:, None, :], window_strides=(1,),
                                 padding=((CONV_W // 2, CONV_W // 2),),
                                 dimension_numbers=('NWC', 'WIO', 'NWC'),
                                 feature_group_count=C_WIDTH)
    return b_gate * y


def expert_choice_moe(h, router, w_gate, w_up, w_down):
    bsz, seq, d = h.shape
    cap = EC_CAPACITY_FACTOR * seq // N_EXPERTS
    aff = jax.nn.softmax(jnp.einsum('bsd,de->bse', h, router).astype(jnp.float32), axis=-1)
    gate, idx = lax.top_k(aff.transpose(0, 2, 1), cap)
    xe = jax.vmap(lambda hb, ib: hb[ib])(h, idx)
    hid = jax.nn.silu(jnp.einsum('becd,edf->becf', xe, w_gate)) * jnp.einsum('becd,edf->becf', xe, w_up)
    ye = jnp.einsum('becf,efd->becd', hid, w_down) * gate[..., None].astype(h.dtype)
    return jax.vmap(lambda ib, yb: jnp.zeros((seq, d), yb.dtype).at[ib.reshape(-1)].add(yb.reshape(-1, d)))(idx, ye)


def setup_inputs(seed: int = 0) -> dict:
    key = jax.random.key(seed)
    ks = jax.random.split(key, 17)
    nrm = jax.random.normal
    f32 = jnp.float32
    return {
        'x': nrm(ks[0], (BATCH, SEQ, D_MODEL), f32),
        'norm_mix': 1.0 + 0.05 * nrm(ks[1], (DEPTH, D_MODEL), f32),
        'norm_ffn': 1.0 + 0.05 * nrm(ks[2], (DEPTH, D_MODEL), f32),
        'norm_final': 1.0 + 0.05 * nrm(ks[3], (D_MODEL,), f32),
        'w_in_ab': nrm(ks[4], (N_EVEN, D_MODEL, IN_AB), f32) * D_MODEL ** -0.5,
        'a_v_norm': 1.0 + 0.05 * nrm(ks[5], (N_EVEN, A_WIDTH), f32),
        'a_spatial_w': nrm(ks[6], (N_EVEN, A_GROUPS, CHUNK, CHUNK), f32) * CHUNK ** -0.5,
        'a_spatial_b': 1.0 + 0.05 * nrm(ks[7], (N_EVEN, A_GROUPS, CHUNK), f32),
        'b_rpb': 0.1 * nrm(ks[8], (N_EVEN, B_HEADS, 2 * NA_ROWS - 1, 2 * NA_COLS - 1), f32),
        'w_out_ab': nrm(ks[9], (N_EVEN, MIX_WIDTH, D_MODEL), f32) * MIX_WIDTH ** -0.5,
        'w_in_cd': nrm(ks[10], (N_ODD, D_MODEL, IN_CD), f32) * D_MODEL ** -0.5,
        'c_conv': nrm(ks[11], (N_ODD, CONV_W, C_WIDTH), f32) * CONV_W ** -0.5,
        'w_out_cd': nrm(ks[12], (N_ODD, MIX_WIDTH, D_MODEL), f32) * MIX_WIDTH ** -0.5,
        'router': nrm(ks[13], (DEPTH, D_MODEL, N_EXPERTS), f32) * D_MODEL ** -0.5,
        'w_gate': nrm(ks[14], (DEPTH, N_EXPERTS, D_MODEL, D_EXPERT), f32) * D_MODEL ** -0.5,
        'w_up': nrm(ks[15], (DEPTH, N_EXPERTS, D_MODEL, D_EXPERT), f32) * D_MODEL ** -0.5,
        'w_down': nrm(ks[16], (DEPTH, N_EXPERTS, D_EXPERT, D_MODEL), f32) * D_EXPERT ** -0.5,
    }


def reference(x, norm_mix, norm_ffn, norm_final, w_in_ab, a_v_norm, a_spatial_w, a_spatial_b,
              b_rpb, w_out_ab, w_in_cd, c_conv, w_out_cd, router, w_gate, w_up, w_down):
    bsz, seq, _ = x.shape
    for layer in range(DEPTH):
        h = rms_norm(x, norm_mix[layer])
        i = layer // 2
        if layer % 2 == 0:
            p = h @ w_in_ab[i]
            a_u = p[..., :A_WIDTH]
            a_v = p[..., A_WIDTH:2 * A_WIDTH]
            o = 2 * A_WIDTH
            b_q = p[..., o:o + B_WIDTH].reshape(bsz, seq, B_HEADS, HEAD_DIM)
            b_k = p[..., o + B_WIDTH:o + 2 * B_WIDTH].reshape(bsz, seq, B_HEADS, HEAD_DIM)
            b_v = p[..., o + 2 * B_WIDTH:o + 3 * B_WIDTH].reshape(bsz, seq, B_HEADS, HEAD_DIM)
            y_a = spatial_gating_unit(a_u, a_v, a_v_norm[i], a_spatial_w[i], a_spatial_b[i])
            y_b = neighbourhood_attention(b_q, b_k, b_v, b_rpb[i])
            x = x + jnp.concatenate([y_a, y_b], axis=-1) @ w_out_ab[i]
        else:
            p = h @ w_in_cd[i]
            c_b = p[..., :C_WIDTH]
            c_c = p[..., C_WIDTH:2 * C_WIDTH]
            c_x = p[..., 2 * C_WIDTH:3 * C_WIDTH]
            o = 3 * C_WIDTH
            d_q = p[..., o:o + D_WIDTH].reshape(bsz, seq, D_HEADS, HEAD_DIM)
            d_k = p[..., o + D_WIDTH:o + 2 * D_WIDTH].reshape(bsz, seq, D_HEADS, HEAD_DIM)
            d_v = p[..., o + 2 * D_WIDTH:o + 3 * D_WIDTH].reshape(bsz, seq, D_HEADS, HEAD_DIM)
            y_c = gated_short_conv(c_b, c_c, c_x, c_conv[i])
            y_d = dilated_attention(d_q, d_k, d_v)
            x = x + jnp.concatenate([y_c, y_d], axis=-1) @ w_out_cd[i]
        x = x + expert_choice_moe(rms_norm(x, norm_ffn[layer]), router[layer], w_gate[layer], w_up[layer], w_down[layer])
    return rms_norm(x, norm_final)
```

```python
import numpy as np
from contextlib import ExitStack
import concourse.bass as bass
import concourse.mybir as mybir
from concourse.bass_utils import run_bass_kernel_spmd

F32 = mybir.dt.float32
BF16 = mybir.dt.bfloat16
I32 = mybir.dt.int32
AF = mybir.ActivationFunctionType
ALU = mybir.AluOpType
AX = mybir.AxisListType

SEM_EPOCH = 30000


class Prog:
    def __init__(self, nc, n_dma_sems=6):
        self.nc = nc
        self.nsem = 0
        self.stack = ExitStack()
        self.root = ExitStack()
        self.scopes = []
        self.eng = {'pe': nc.tensor, 'act': nc.scalar, 'dve': nc.vector,
                    'pool': nc.gpsimd, 'sp': nc.sync}
        self.sem = {}
        self.cnt = {}
        for k in ['pe', 'act', 'dve', 'pool']:
            self.sem[k] = self._newsem("s_" + k)
            self.cnt[k] = 0
        self.waited = {e: {} for e in self.eng}
        self.last_w = {}
        self.readers = {}
        self.dsem = {}
        self.dsem_i = {}
        self.n_dma_sems = n_dma_sems

    def _newsem(self, name):
        self.nsem = getattr(self, 'nsem', 0) + 1
        return self.root.enter_context(self.nc.semaphore(name + "_%d" % self.nsem))

    def sb(self, name, shape, dtype):
        self.nsem += 1
        return self.stack.enter_context(self.nc.sbuf_tensor("sb_%s_%d" % (name, self.nsem), shape, dtype))

    def ps(self, name, shape, dtype=F32):
        self.nsem += 1
        return self.stack.enter_context(self.nc.psum_tensor("ps_%s_%d" % (name, self.nsem), shape, dtype))

    def _deps(self, reads, writes):
        deps = []
        for r in reads:
            if r in self.last_w:
                deps.append(self.last_w[r])
        for w in writes:
            if w in self.last_w:
                deps.append(self.last_w[w])
            deps.extend(self.readers.get(w, ()))
        return deps

    def _emit_waits(self, engine, deps):
        e = self.eng[engine]
        wd = self.waited[engine]
        best = {}
        for (sem, val, src) in deps:
            if src == 'pe' and engine == 'pe':
                continue
            k = id(sem)
            if wd.get(k, 0) >= val:
                continue
            if k not in best or best[k][1] < val:
                best[k] = (sem, val)
        for k, (sem, val) in best.items():
            e.wait_ge(sem, val)
            wd[k] = val

    def _record(self, ev, reads, writes):
        for w in writes:
            self.last_w[w] = ev
            self.readers[w] = []
        for r in reads:
            if r not in writes:
                self.readers.setdefault(r, []).append(ev)

    def op(self, engine, fn, reads=(), writes=()):
        deps = self._deps(reads, writes)
        self._emit_waits(engine, deps)
        ins = fn(self.eng[engine])
        if self.cnt[engine] >= SEM_EPOCH:
            self.sem[engine] = self._newsem("s_" + engine)
            self.cnt[engine] = 0
        self.cnt[engine] += 1
        ins.then_inc(self.sem[engine], 1)
        ev = (self.sem[engine], self.cnt[engine], engine)
        self._record(ev, reads, writes)
        return ev

    def dma(self, queue, out, in_=None, reads=(), writes=(), **kw):
        deps = self._deps(reads, writes)
        if queue not in self.dsem:
            self.dsem[queue] = [[self._newsem("d_" + queue), 0] for _ in range(self.n_dma_sems)]
            self.dsem_i[queue] = 0
        i = self.dsem_i[queue]
        self.dsem_i[queue] = (i + 1) % self.n_dma_sems
        slot = self.dsem[queue][i]
        if slot[1] >= SEM_EPOCH:
            deps.append((slot[0], slot[1], None))
            slot[0] = self._newsem("d_" + queue)
            slot[1] = 0
        if slot[1] > 0:
            deps.append((slot[0], slot[1], None))
        self._emit_waits(queue, deps)
        if callable(out):
            ins = out(self.eng[queue])
        else:
            ins = self.eng[queue].dma_start(out=out, in_=in_, **kw)
        slot[1] += 16
        ins.then_inc(slot[0], 16)
        ev = (slot[0], slot[1], None)
        self._record(ev, reads, writes)
        return ev

    def barrier(self):
        evs = [(self.sem[e], self.cnt[e], e) for e in self.sem if self.cnt[e] > 0]
        for q in self.dsem:
            for slot in self.dsem[q]:
                if slot[1] > 0:
                    evs.append((slot[0], slot[1], None))
        for x in self.eng:
            e = self.eng[x]
            wd = self.waited[x]
            for (sem, val, src) in evs:
                if wd.get(id(sem), 0) >= val:
                    continue
                e.wait_ge(sem, val)
                wd[id(sem)] = val
        self.last_w = {}
        self.readers = {}

    def push(self):
        self.scopes.append(self.stack)
        self.stack = ExitStack()

    def pop(self):
        self.barrier()
        self.stack.close()
        self.stack = self.scopes.pop()

    def finish(self, out_keys, engine='sp'):
        deps = [self.last_w[k] for k in out_keys]
        self._emit_waits(engine, deps)
        self.stack.close()
        self.root.close()


S = 2048
D = 2048
NT = 16
KC = 16
EPS_RMS = 1e-6
EPS_LN = 1e-5
NEG = -30000.0
NEXP = 16
CAP = 256


def _na_chunks(m):
    rows = S // 64
    rs = lambda r: int(np.clip(r - 4, 0, rows - 8))
    lo = rs(2 * m) // 2
    hi = (rs(2 * m + 1) + 7) // 2
    return list(range(lo, hi + 1))


def _na_tile_index():
    idx = {}
    n = 0
    for rel in range(-2, 3):
        idx[('mid', rel)] = n
        n += 1
    for m in (0, 1, 14, 15):
        for j in _na_chunks(m):
            idx[(m, j)] = n
            n += 1
    return idx, n


def _na_tile_id(idx, m, j):
    if 2 <= m <= 13:
        return idx[('mid', j - m)]
    return idx[(m, j)]


def na_bias_table(rpb):
    idx, n = _na_tile_index()
    H = rpb.shape[0]
    rows = S // 64
    tab = np.full((H, 128, n, 128), NEG, np.float32)
    done = set()
    for m in range(16):
        for j in _na_chunks(m):
            t = _na_tile_id(idx, m, j)
            if t in done:
                continue
            done.add(t)
            kk = np.arange(128)
            qq = np.arange(128)
            krow = 2 * j + kk // 64
            kcol = kk % 64
            qrow = 2 * m + qq // 64
            qcol = qq % 64
            rs = np.clip(qrow - 4, 0, rows - 8)
            cs = np.clip(qcol - 8, 0, 64 - 16)
            valid = ((krow[:, None] >= rs[None, :]) & (krow[:, None] < rs[None, :] + 8) &
                     (kcol[:, None] >= cs[None, :]) & (kcol[:, None] < cs[None, :] + 16))
            dr = np.clip(krow[:, None] - qrow[None, :] + 7, 0, 14)
            dc = np.clip(kcol[:, None] - qcol[None, :] + 15, 0, 30)
            g = rpb[:, dr, dc]
            tab[:, :, t, :] = np.where(valid[None], g, np.float32(NEG))
    return tab


def dil_bias_table():
    H = 8
    slopes = np.array([2.0 ** (-8.0 * (h + 1) / H) for h in range(H)], np.float64)
    tab = np.full((H, 128, 17, 128), NEG, np.float32)
    kk = np.arange(128)[:, None]
    qq = np.arange(128)[None, :]
    for rel in range(-8, 9):
        delta = rel * 128 + kk - qq
        ad = np.abs(delta)
        mult = np.zeros_like(ad)
        for (w, dil) in ((128, 1), (512, 4), (2048, 16)):
            rad = w // (2 * dil)
            mult += ((ad % dil == 0) & (ad // dil <= rad)).astype(ad.dtype)
        for h in range(H):
            v = np.where(mult > 0, np.log(np.maximum(mult, 1)) - slopes[h] * ad, NEG)
            tab[h, :, rel + 8, :] = v.astype(np.float32)
    return tab


class MK:
    def __init__(self, nlayers=2, stop=None, debug=False, start=0):
        self.stop = stop
        self.start = start
        nc = bass.Bass("TRN2", target_bir_lowering=False)
        self.nc = nc
        self.P = Prog(nc)
        P = self.P
        self.declared = []

        def di(name, shape, dt=F32):
            self.declared.append(name)
            return nc.dram_tensor(name, shape, dt, kind="ExternalInput").ap()
        ds = lambda name, shape, dt=F32: nc.dram_tensor(name, shape, dt, kind="Internal").ap()
        self.x_in = di("x", [S, D])
        self.norm_mix = di("norm_mix", [2, D])
        self.norm_ffn = di("norm_ffn", [2, D])
        self.norm_final = di("norm_final", [D])
        self.w_in_ab = di("w_in_ab", [D, 5120])
        self.lng = di("a_v_norm", [1024])
        self.wsT = di("a_spatial_wT", [128, 8, 128])
        self.bsT = di("a_spatial_bT", [128, 8])
        self.na_tab = di("na_tab", [8, 128, 21, 128])
        self.w_out_ab = di("w_out_ab", [D, D])
        self.w_in_cd = di("w_in_cd", [D, 6144])
        self.taps = di("c_convT", [128, 8, 3])
        self.dil_tab = di("dil_tab", [8, 128, 17, 128])
        self.w_out_cd = di("w_out_cd", [D, D])
        self.router = di("router", [2, D, NEXP])
        if stop not in ('mix0', 'mix1only'):
            self.w_gate = di("w_gate", [2, NEXP, D, D])
            self.w_up = di("w_up", [2, NEXP, D, D])
            self.w_down = di("w_down", [2, NEXP, D, D])
        self.ident_in = di("ident", [128, 128])
        self.iota_in = di("iota", [128, 256])
        self.tokidx_in = di("tokidx", [128, NT])
        self.out = nc.dram_tensor("out", [S, D], F32, kind="ExternalOutput").ap()
        self.xs = ds("xs", [S, D])
        self.h2d = ds("h2d", [S, D])
        self.yTd = ds("yTd", [NT, 128, 8, 128], BF16)
        self.QTd = ds("QTd", [8, 128, S], BF16)
        self.KTd = ds("KTd", [8, 128, S], BF16)
        self.Vd = ds("Vd", [S, 1024], BF16)
        self.ring = [P.sb("ring%d" % i, [128, KC, 512], BF16) for i in range(4)]
        self.ring_n = 0
        self.ident = P.sb("ident", [128, 128], F32)
        self.ones_bf = P.sb("ones_bf", [128, 128], BF16)
        self.pb = [P.ps("pb%d" % i, [128, 512], F32) for i in range(8)]
        self.pb_n = 0
        P.dma('sp', self.ident[:], self.ident_in, writes=['ident'])
        P.op('dve', lambda e: e.memset(self.ones_bf[:], 1.0), writes=['ones_bf'])

        if start == 0:
            self.layer_mix(0)
            if stop == 'mix0':
                return self.finish(self.xs)
            self.moe(0)
            if stop == 'moe0':
                return self.finish(self.xs)
        self.layer_mix(1)
        if stop in ('mix1', 'mix1only'):
            return self.finish(self.xs)
        self.moe(1)
        if stop == 'moe1':
            return self.finish(self.xs)
        self.norm_stage(self.xs, self.norm_final, 'final')
        P.finish([])

    def finish(self, src):
        P = self.P
        P.push()
        t = P.sb("cp", [128, D], F32)
        for tt in range(NT):
            P.dma('sp', t[:], src[tt * 128:(tt + 1) * 128, :], reads=['xs'], writes=['cp'])
            P.dma('sp', self.out[tt * 128:(tt + 1) * 128, :], t[:], reads=['cp'], writes=['out'])
        P.pop()
        P.finish([])

    def bank(self):
        i = self.pb_n % 8
        self.pb_n += 1
        return i

    def load_slab(self, w_ap, col0):
        i = self.ring_n % 4
        self.ring_n += 1
        src = w_ap[:, col0:col0 + 512].rearrange("(kc p) f -> p kc f", p=128)
        self.P.dma('pool', self.ring[i][:], src, writes=['ring%d' % i])
        return i

    def norm_stage(self, src, gvec, mode, layer=0):
        P = self.P
        P.push()
        gb = P.sb("gb", [128, D], F32)
        P.dma('sp', gb[:], gvec.partition_broadcast(128), writes=['gb'])
        xb = [P.sb("xb%d" % i, [128, D], F32) for i in range(2)]
        hb = [P.sb("hb%d" % i, [128, D], F32) for i in range(2)]
        ss = P.sb("ss", [128, 4], F32)
        if mode == 'moe':
            rt = P.sb("rt", [128, KC, NEXP], F32)
            P.dma('sp', rt[:], self.router[layer].rearrange("(kc p) e -> p kc e", p=128), writes=['rt'])
            hT32 = P.sb("hT32", [128, KC, 128], F32)
            sm = P.sb("sm", [128, 8], F32)
            ex = P.sb("ex", [128, NEXP], F32)
        for tt in range(NT):
            x_t = xb[tt % 2]
            h_t = hb[tt % 2]
            kx, kh = 'xb%d' % (tt % 2), 'hb%d' % (tt % 2)
            P.dma('sp', x_t[:], src[tt * 128:(tt + 1) * 128, :], reads=['xs'], writes=[kx])
            P.op('act', lambda e: e.activation(out=h_t[:], in_=x_t[:], func=AF.Square, accum_out=ss[:, 0:1]),
                 reads=[kx], writes=[kh, 'ss'])
            P.op('act', lambda e: e.activation(out=ss[:, 1:2], in_=ss[:, 0:1], func=AF.Sqrt, scale=1.0 / D, bias=EPS_RMS),
                 reads=['ss'], writes=['ss'])
            P.op('dve', lambda e: e.reciprocal(out=ss[:, 2:3], in_=ss[:, 1:2]), reads=['ss'], writes=['ss2'])
            P.op('dve', lambda e: e.scalar_tensor_tensor(out=h_t[:], in0=x_t[:], scalar=ss[:, 2:3], in1=gb[:],
                                                         op0=ALU.mult, op1=ALU.mult),
                 reads=[kx, 'ss2', 'gb', kh], writes=[kh])
            if mode == 'final':
                P.dma('sp', self.out[tt * 128:(tt + 1) * 128, :], h_t[:], reads=[kh], writes=['out'])
                continue
            if mode == 'moe':
                P.dma('sp', self.h2d[tt * 128:(tt + 1) * 128, :], h_t[:], reads=[kh], writes=['h2d'])
            for b4 in range(4):
                bi = self.bank()
                kb = 'pb%d' % bi
                for j in range(4):
                    kc = b4 * 4 + j
                    P.op('pe', lambda e, kc=kc, j=j: e.transpose(out=self.pb[bi][:, j * 128:(j + 1) * 128],
                                                                in_=h_t[:, kc * 128:(kc + 1) * 128],
                                                                identity=self.ident[:]),
                         reads=[kh, 'ident'], writes=[kb])
                if mode == 'mix':
                    dst = self.hT[:, b4 * 4:(b4 + 1) * 4, tt * 128:(tt + 1) * 128]
                    wk = 'hT'
                else:
                    dst = hT32[:, b4 * 4:(b4 + 1) * 4, :]
                    wk = 'hT32'
                srcp = self.pb[bi][:].rearrange("p (j t) -> p j t", j=4)
                if b4 % 2 == 0:
                    P.op('act', lambda e: e.copy(out=dst, in_=srcp), reads=[kb], writes=[wk])
                else:
                    P.op('dve', lambda e: e.tensor_copy(out=dst, in_=srcp), reads=[kb], writes=[wk])
            if mode == 'moe':
                bi = self.bank()
                kb = 'pb%d' % bi
                for kc in range(KC):
                    P.op('pe', lambda e, kc=kc: e.matmul(out=self.pb[bi][:, 0:NEXP], lhsT=hT32[:, kc, :], rhs=rt[:, kc, :],
                                                        start=(kc == 0), stop=(kc == KC - 1)),
                         reads=['hT32', 'rt'], writes=[kb])
                lg = self.pb[bi][:, 0:NEXP]
                P.op('dve', lambda e: e.tensor_reduce(out=sm[:, 0:1], in_=lg, op=ALU.max, axis=AX.X, negate=True),
                     reads=[kb], writes=['sm'])
                P.op('act', lambda e: e.activation(out=ex[:], in_=lg, func=AF.Exp, bias=sm[:, 0:1], accum_out=sm[:, 1:2]),
                     reads=[kb, 'sm'], writes=['ex', 'sm'])
                P.op('dve', lambda e: e.reciprocal(out=sm[:, 2:3], in_=sm[:, 1:2]), reads=['sm'], writes=['sm2'])
                P.op('dve', lambda e: e.tensor_scalar(out=self.aff[:, tt, :], in0=ex[:], scalar1=sm[:, 2:3], scalar2=None,
                                                      op0=ALU.mult),
                     reads=['ex', 'sm2'], writes=['aff'])
        P.pop()

    def layer_mix(self, layer):
        P = self.P
        src = self.x_in if layer == self.start else self.xs
        w_in = self.w_in_ab if layer == 0 else self.w_in_cd
        w_out = self.w_out_ab if layer == 0 else self.w_out_cd
        P.push()
        if layer == 1:
            self.yaT = P.sb("yaT", [128, 8, S], BF16)
        P.push()
        self.hT = P.sb("hT", [128, KC, S], BF16)
        self.norm_stage(src, self.norm_mix[layer], 'mix')
        if layer == 0:
            self.sgu_stage(w_in)
            qkv0 = 2048
        else:
            self.conv_stage(w_in)
            qkv0 = 3072
        self.qkv_stage(w_in, qkv0)
        P.pop()
        self.ybT = P.sb("ybT", [128, 8, S], BF16)
        wslots = [self.load_slab(w_out, db * 512) for db in range(4)]
        self.attn_stage(layer)
        self.outproj_stage(layer, src, wslots)
        P.pop()

    def gelu_tanh(self, pbank, kb, dst, kdst, tmp, ktmp):
        P = self.P
        P.op('act', lambda e: e.activation(out=tmp, in_=pbank, func=AF.Square), reads=[kb], writes=[ktmp])
        P.op('dve', lambda e: e.tensor_scalar(out=tmp, in0=tmp, scalar1=0.044715, scalar2=1.0, op0=ALU.mult, op1=ALU.add),
             reads=[ktmp], writes=[ktmp])
        P.op('dve', lambda e: e.tensor_tensor(out=tmp, in0=tmp, in1=pbank, op=ALU.mult), reads=[ktmp, kb], writes=[ktmp])
        P.op('act', lambda e: e.activation(out=tmp, in_=tmp, func=AF.Sigmoid, scale=1.5957691216057308),
             reads=[ktmp], writes=[ktmp])
        P.op('dve', lambda e: e.tensor_tensor(out=dst, in0=tmp, in1=pbank, op=ALU.mult), reads=[ktmp, kb], writes=[kdst])

    def sgu_stage(self, w_in):
        P = self.P
        P.push()
        slots = [self.load_slab(w_in, c * 512) for c in range(4)]
        lngb = P.sb("lngb", [128, 1024], F32)
        P.dma('sp', lngb[:], self.lng.partition_broadcast(128), writes=['lngb'])
        wsT = P.sb("wsT", [128, 8, 128], BF16)
        P.dma('pool', wsT[:], self.wsT, writes=['wsT'])
        bsT = P.sb("bsT", [128, 8], F32)
        P.dma('sp', bsT[:], self.bsT, writes=['bsT'])
        ut = P.sb("ut", [128, 1024], F32)
        vf = P.sb("vf", [128, 1024], F32)
        vn = P.sb("vn", [128, 1024], BF16)
        tmp = [P.sb("gtmp%d" % i, [128, 512], F32) for i in range(2)]
        ya = P.sb("ya", [128, 1024], F32)
        st = P.sb("bnst", [128, 2, 6], F32)
        mv = P.sb("bnmv", [128, 4], F32)
        yst = [P.sb("yst%d" % i, [128, 8, 128], BF16) for i in range(2)]
        for tt in range(NT):
            banks = []
            for s4 in range(4):
                bi = self.bank()
                banks.append(bi)
                for kc in range(KC):
                    P.op('pe', lambda e, kc=kc: e.matmul(out=self.pb[bi][:], lhsT=self.hT[:, kc, tt * 128:(tt + 1) * 128],
                                                        rhs=self.ring[slots[s4]][:, kc, :], start=(kc == 0), stop=(kc == KC - 1)),
                         reads=['hT', 'ring%d' % slots[s4]], writes=['pb%d' % bi])
            for s4 in range(4):
                bi = banks[s4]
                dstt, kd = (ut, 'ut') if s4 < 2 else (vf, 'vf')
                c0 = (s4 % 2) * 512
                self.gelu_tanh(self.pb[bi][:], 'pb%d' % bi, dstt[:, c0:c0 + 512], kd, tmp[s4 % 2][:], 'gtmp%d' % (s4 % 2))
            for hh in range(2):
                P.op('dve', lambda e, hh=hh: e.bn_stats(out=st[:, hh, :], in_=vf[:, hh * 512:(hh + 1) * 512]),
                     reads=['vf'], writes=['bnst'])
            P.op('dve', lambda e: e.bn_aggr(out=mv[:, 0:2], in_=st[:].rearrange("p a b -> p (a b)")), reads=['bnst'], writes=['bnmv'])
            P.op('act', lambda e: e.activation(out=mv[:, 2:3], in_=mv[:, 1:2], func=AF.Sqrt, bias=EPS_LN), reads=['bnmv'], writes=['bnmv2'])
            P.op('dve', lambda e: e.reciprocal(out=mv[:, 3:4], in_=mv[:, 2:3]), reads=['bnmv2'], writes=['bnmv3'])
            P.op('dve', lambda e: e.tensor_scalar(out=vf[:], in0=vf[:], scalar1=mv[:, 0:1], scalar2=mv[:, 3:4],
                                                  op0=ALU.subtract, op1=ALU.mult), reads=['vf', 'bnmv', 'bnmv3'], writes=['vf'])
            P.op('dve', lambda e: e.tensor_tensor(out=vn[:], in0=vf[:], in1=lngb[:], op=ALU.mult), reads=['vf', 'lngb'], writes=['vn'])
            sb_ = [self.bank(), self.bank()]
            for g in range(8):
                bi = sb_[g // 4]
                P.op('pe', lambda e, g=g: e.matmul(out=self.pb[bi][:, (g % 4) * 128:(g % 4 + 1) * 128], lhsT=wsT[:, g, :],
                                                   rhs=vn[:, g * 128:(g + 1) * 128], start=True, stop=True),
                     reads=['wsT', 'vn'], writes=['pb%d' % bi])
            for hh in range(2):
                bi = sb_[hh]
                yv = ya[:, hh * 512:(hh + 1) * 512].rearrange("p (g c) -> p g c", g=4)
                P.op('dve', lambda e: e.tensor_tensor(out=yv, in0=self.pb[bi][:].rearrange("p (g c) -> p g c", g=4),
                                                      in1=bsT[:, hh * 4:(hh + 1) * 4].unsqueeze(2).to_broadcast([128, 4, 128]), op=ALU.add),
                     reads=['pb%d' % bi, 'bsT'], writes=['ya'])
            P.op('dve', lambda e: e.tensor_tensor(out=ya[:], in0=ya[:], in1=ut[:], op=ALU.mult), reads=['ya', 'ut'], writes=['ya'])
            ys = yst[tt % 2]
            ky = 'yst%d' % (tt % 2)
            for hh in range(2):
                bi = self.bank()
                for j in range(4):
                    c = hh * 4 + j
                    P.op('pe', lambda e, c=c, j=j: e.transpose(out=self.pb[bi][:, j * 128:(j + 1) * 128], in_=ya[:, c * 128:(c + 1) * 128],
                                                              identity=self.ident[:]), reads=['ya', 'ident'], writes=['pb%d' % bi])
                P.op('act', lambda e: e.copy(out=ys[:, hh * 4:(hh + 1) * 4, :], in_=self.pb[bi][:].rearrange("p (j t) -> p j t", j=4)),
                     reads=['pb%d' % bi], writes=[ky])
            P.dma('sp', self.yTd[tt], ys[:], reads=[ky], writes=['yTd'])
        P.pop()

    def qkv_stage(self, w_in, col0):
        P = self.P
        P.push()
        stg = [P.sb("qstg%d" % i, [128, S], BF16) for i in range(2)]
        vst = [P.sb("vstg%d" % i, [128, 512], BF16) for i in range(2)]
        n = 0
        for which in range(2):
            dstd = self.QTd if which == 0 else self.KTd
            scale = (128.0 ** -0.5) if which == 0 else 1.0
            for sl in range(2):
                slot = self.load_slab(w_in, col0 + which * 1024 + sl * 512)
                for fc in range(4):
                    h = sl * 4 + fc
                    sg = stg[n % 2]
                    ks = 'qstg%d' % (n % 2)
                    n += 1
                    for tb in range(4):
                        bi = self.bank()
                        for kc in range(KC):
                            P.op('pe', lambda e, kc=kc: e.matmul(out=self.pb[bi][:], lhsT=self.ring[slot][:, kc, fc * 128:(fc + 1) * 128],
                                                                rhs=self.hT[:, kc, tb * 512:(tb + 1) * 512], start=(kc == 0), stop=(kc == KC - 1)),
                                 reads=['hT', 'ring%d' % slot], writes=['pb%d' % bi])
                        if tb % 2 == 0:
                            P.op('act', lambda e: e.activation(out=sg[:, tb * 512:(tb + 1) * 512], in_=self.pb[bi][:], func=AF.Copy, scale=scale),
                                 reads=['pb%d' % bi], writes=[ks])
                        else:
                            P.op('dve', lambda e: e.tensor_scalar(out=sg[:, tb * 512:(tb + 1) * 512], in0=self.pb[bi][:], scalar1=scale, scalar2=None, op0=ALU.mult),
                                 reads=['pb%d' % bi], writes=[ks])
                    P.dma('sp', dstd[h], sg[:], reads=[ks], writes=['QKd'])
        m = 0
        for sl in range(2):
            slot = self.load_slab(w_in, col0 + 2048 + sl * 512)
            for tt in range(NT):
                bi = self.bank()
                for kc in range(KC):
                    P.op('pe', lambda e, kc=kc: e.matmul(out=self.pb[bi][:], lhsT=self.hT[:, kc, tt * 128:(tt + 1) * 128],
                                                        rhs=self.ring[slot][:, kc, :], start=(kc == 0), stop=(kc == KC - 1)),
                         reads=['hT', 'ring%d' % slot], writes=['pb%d' % bi])
                vs = vst[m % 2]
                kv = 'vstg%d' % (m % 2)
                m += 1
                if tt % 2 == 0:
                    P.op('act', lambda e: e.copy(out=vs[:], in_=self.pb[bi][:]), reads=['pb%d' % bi], writes=[kv])
                else:
                    P.op('dve', lambda e: e.tensor_copy(out=vs[:], in_=self.pb[bi][:]), reads=['pb%d' % bi], writes=[kv])
                P.dma('sp', self.Vd[tt * 128:(tt + 1) * 128, sl * 512:(sl + 1) * 512], vs[:], reads=[kv], writes=['Vd'])
        P.pop()

    def attn_stage(self, layer):
        P = self.P
        P.push()
        if layer == 0:
            idx, ntile = _na_tile_index()
            tab = self.na_tab
            chunks = lambda i: [(j, _na_tile_id(idx, i, j)) for j in _na_chunks(i)]
        else:
            ntile = 17
            tab = self.dil_tab
            chunks = lambda i: [(j, j - i + 8) for j in range(max(0, i - 8), min(15, i + 8) + 1)]
        QT = [P.sb("aQT%d" % i, [128, S], BF16) for i in range(2)]
        KT = [P.sb("aKT%d" % i, [128, S], BF16) for i in range(2)]
        V = [P.sb("aV%d" % i, [128, NT, 128], BF16) for i in range(2)]
        Tb = [P.sb("aTb%d" % i, [128, ntile, 128], F32) for i in range(2)]
        E = [P.sb("aE%d" % i, [128, ntile, 128], BF16) for i in range(2)]
        pexp = [P.sb("apexp%d" % i, [128, 4, 128], BF16) for i in range(2)]
        pt = [P.sb("apt%d" % i, [128, 4, 128], BF16) for i in range(3)]
        rden = [P.sb("arden%d" % i, [128, 128], F32) for i in range(2)]
        nst = 0
        nq = 0
        nb_ = 0
        for h in range(8):
            hp = h % 2
            kq, kk, kv, kt, ke = 'aQT%d' % hp, 'aKT%d' % hp, 'aV%d' % hp, 'aTb%d' % hp, 'aE%d' % hp
            P.dma('sp', QT[hp][:], self.QTd[h], reads=['QKd'], writes=[kq])
            P.dma('sp', KT[hp][:], self.KTd[h], reads=['QKd'], writes=[kk])
            P.dma('sp', V[hp][:], self.Vd[:, h * 128:(h + 1) * 128].rearrange("(t p) c -> p t c", p=128), reads=['Vd'], writes=[kv])
            P.dma('sp', Tb[hp][:], tab[h], writes=[kt])
            P.op('act', lambda e: e.activation(out=E[hp][:], in_=Tb[hp][:], func=AF.Exp), reads=[kt], writes=[ke])
            for i in range(NT):
                cl = chunks(i)
                bo = 4 + (nq % 2)
                bd = 6 + (nq % 2)
                rd = rden[nq % 2]
                krd = 'arden%d' % (nq % 2)
                nq += 1
                ncl = len(cl)
                done = 0
                for b0 in range(0, ncl, 4):
                    batch = cl[b0:b0 + 4]
                    nb = len(batch)
                    bi = nst % 4
                    nst += 1
                    kb = 'pb%d' % bi
                    for jj, (j, tid) in enumerate(batch):
                        P.op('pe', lambda e, jj=jj, j=j: e.matmul(out=self.pb[bi][:, jj * 128:(jj + 1) * 128], lhsT=KT[hp][:, j * 128:(j + 1) * 128],
                                                                  rhs=QT[hp][:, i * 128:(i + 1) * 128], start=True, stop=True),
                             reads=[kq, kk], writes=[kb])
                    pe_ = pexp[nb_ % 2]
                    kpe = 'apexp%d' % (nb_ % 2)
                    p_t = pt[nb_ % 3]
                    kpt = 'apt%d' % (nb_ % 3)
                    nb_ += 1
                    P.op('act', lambda e: e.activation(out=pe_[:, 0:nb, :], in_=self.pb[bi][:, 0:nb * 128].rearrange("p (j t) -> p j t", j=nb), func=AF.Exp),
                         reads=[kb], writes=[kpe])
                    t0 = batch[0][1]
                    P.op('dve', lambda e: e.tensor_tensor(out=p_t[:, 0:nb, :], in0=pe_[:, 0:nb, :], in1=E[hp][:, t0:t0 + nb, :], op=ALU.mult),
                         reads=[kpe, ke], writes=[kpt])
                    for jj, (j, tid) in enumerate(batch):
                        first = (done == 0)
                        last = (done == ncl - 1)
                        done += 1
                        P.op('pe', lambda e, jj=jj, j=j: e.matmul(out=self.pb[bo][:, 0:128], lhsT=V[hp][:, j, :], rhs=p_t[:, jj, :], start=first, stop=last),
                             reads=[kv, kpt], writes=['pb%d' % bo])
                        P.op('pe', lambda e, jj=jj, j=j: e.matmul(out=self.pb[bd][:, 0:128], lhsT=self.ones_bf[:], rhs=p_t[:, jj, :], start=first, stop=last),
                             reads=['ones_bf', kpt], writes=['pb%d' % bd])
                P.op('dve', lambda e: e.reciprocal(out=rd[:], in_=self.pb[bd][:, 0:128]), reads=['pb%d' % bd], writes=[krd])
                P.op('dve', lambda e: e.tensor_tensor(out=self.ybT[:, h, i * 128:(i + 1) * 128], in0=self.pb[bo][:, 0:128], in1=rd[:], op=ALU.mult),
                     reads=['pb%d' % bo, krd], writes=['ybT'])
        P.pop()

    def outproj_stage(self, layer, src, wslots):
        P = self.P
        P.push()
        xb = [P.sb("oxb%d" % i, [128, D], F32) for i in range(2)]
        xo = [P.sb("oxo%d" % i, [128, D], F32) for i in range(2)]
        yt = [P.sb("oyt%d" % i, [128, 8, 128], BF16) for i in range(2)]
        self.pb_n = 0
        for tt in range(NT):
            p2 = tt % 2
            P.dma('sp', xb[p2][:], src[tt * 128:(tt + 1) * 128, :], reads=['xs'], writes=['oxb%d' % p2])
            if layer == 0:
                P.dma('sp', yt[p2][:], self.yTd[tt], reads=['yTd'], writes=['oyt%d' % p2])
            for db in range(4):
                bi = self.bank()
                for kc in range(KC):
                    if kc < 8:
                        if layer == 0:
                            lhsT, kl = yt[p2][:, kc, :], 'oyt%d' % p2
                        else:
                            lhsT, kl = self.yaT[:, kc, tt * 128:(tt + 1) * 128], 'yaT'
                    else:
                        lhsT, kl = self.ybT[:, kc - 8, tt * 128:(tt + 1) * 128], 'ybT'
                    P.op('pe', lambda e, kc=kc, lhsT=lhsT: e.matmul(out=self.pb[bi][:], lhsT=lhsT, rhs=self.ring[wslots[db]][:, kc, :],
                                                                   start=(kc == 0), stop=(kc == KC - 1)),
                         reads=[kl, 'ring%d' % wslots[db]], writes=['pb%d' % bi])
                P.op('dve', lambda e: e.tensor_tensor(out=xo[p2][:, db * 512:(db + 1) * 512], in0=self.pb[bi][:], in1=xb[p2][:, db * 512:(db + 1) * 512], op=ALU.add),
                     reads=['pb%d' % bi, 'oxb%d' % p2], writes=['oxo%d' % p2])
            P.dma('sp', self.xs[tt * 128:(tt + 1) * 128, :], xo[p2][:], reads=['oxo%d' % p2], writes=['xs_w'])
        P.pop()

    def conv_stage(self, w_in):
        P = self.P
        P.push()
        taps = P.sb("taps", [128, 8, 3], F32)
        P.dma('sp', taps[:], self.taps, writes=['taps'])
        z = P.sb("cz", [128, S + 2], F32)
        yv = P.sb("cyv", [128, S], F32)
        csb = [P.sb("ccsb%d" % i, [128, 512], F32) for i in range(2)]
        P.op('dve', lambda e: e.memset(z[:], 0.0), writes=['cz'])
        n = 0
        for hf in range(2):
            slab_c = self.load_slab(w_in, 1024 + hf * 512)
            slab_x = self.load_slab(w_in, 2048 + hf * 512)
            slab_b = self.load_slab(w_in, hf * 512)
            for cc4 in range(4):
                cc = hf * 4 + cc4
                for tb in range(4):
                    bc = self.bank()
                    bx = self.bank()
                    for (bi, slab) in ((bc, slab_c), (bx, slab_x)):
                        for kc in range(KC):
                            P.op('pe', lambda e, kc=kc, bi=bi, slab=slab: e.matmul(out=self.pb[bi][:], lhsT=self.ring[slab][:, kc, cc4 * 128:(cc4 + 1) * 128],
                                                                                  rhs=self.hT[:, kc, tb * 512:(tb + 1) * 512], start=(kc == 0), stop=(kc == KC - 1)),
                                 reads=['hT', 'ring%d' % slab], writes=['pb%d' % bi])
                    cs = csb[n % 2]
                    kcs = 'ccsb%d' % (n % 2)
                    n += 1
                    P.op('act', lambda e: e.copy(out=cs[:], in_=self.pb[bc][:]), reads=['pb%d' % bc], writes=[kcs])
                    P.op('dve', lambda e: e.tensor_tensor(out=z[:, 1 + tb * 512:1 + (tb + 1) * 512], in0=cs[:], in1=self.pb[bx][:], op=ALU.mult),
                         reads=[kcs, 'pb%d' % bx], writes=['cz'])
                P.op('dve', lambda e: e.tensor_scalar(out=yv[:], in0=z[:, 0:S], scalar1=taps[:, cc, 0:1], scalar2=None, op0=ALU.mult),
                     reads=['cz', 'taps'], writes=['cyv'])
                P.op('dve', lambda e: e.scalar_tensor_tensor(out=yv[:], in0=z[:, 1:S + 1], scalar=taps[:, cc, 1:2], in1=yv[:], op0=ALU.mult, op1=ALU.add),
                     reads=['cz', 'taps', 'cyv'], writes=['cyv'])
                P.op('dve', lambda e: e.scalar_tensor_tensor(out=yv[:], in0=z[:, 2:S + 2], scalar=taps[:, cc, 2:3], in1=yv[:], op0=ALU.mult, op1=ALU.add),
                     reads=['cz', 'taps', 'cyv'], writes=['cyv'])
                for tb in range(4):
                    bb = self.bank()
                    for kc in range(KC):
                        P.op('pe', lambda e, kc=kc: e.matmul(out=self.pb[bb][:], lhsT=self.ring[slab_b][:, kc, cc4 * 128:(cc4 + 1) * 128],
                                                            rhs=self.hT[:, kc, tb * 512:(tb + 1) * 512], start=(kc == 0), stop=(kc == KC - 1)),
                             reads=['hT', 'ring%d' % slab_b], writes=['pb%d' % bb])
                    P.op('dve', lambda e: e.tensor_tensor(out=self.yaT[:, cc, tb * 512:(tb + 1) * 512], in0=self.pb[bb][:], in1=yv[:, tb * 512:(tb + 1) * 512], op=ALU.mult),
                         reads=['pb%d' % bb, 'cyv'], writes=['yaT'])
        P.pop()

    def moe(self, layer):
        P = self.P
        P.push()
        self.aff = P.sb("aff", [128, NT, NEXP], F32)
        mask_tm = P.sb("mask_tm", [128, NT, NEXP], F32)
        pos_tm = P.sb("pos_tm", [128, NT, NEXP], F32)
        rg = P.sb("rg", [128, NT, NEXP, 2], F32)
        iota = P.sb("iota", [128, 256], F32)
        tokidx = P.sb("tokidx", [128, NT], F32)
        P.dma('sp', iota[:], self.iota_in, writes=['iota'])
        P.dma('sp', tokidx[:], self.tokidx_in, writes=['tokidx'])
        wg, wu, wd = self.w_gate[layer], self.w_up[layer], self.w_down[layer]
        specs = []
        for ex_ in range(NEXP):
            for fs in range(4):
                specs.append((wg[ex_], fs * 512))
                specs.append((wu[ex_], fs * 512))
            for ds_ in range(4):
                specs.append((wd[ex_], ds_ * 512))
        sched = {'issued': 0, 'taken': 0, 'slots': []}

        def issue():
            n = sched['issued']
            if n < len(specs):
                sched['slots'].append(self.load_slab(*specs[n]))
                sched['issued'] = n + 1

        def take():
            sl = sched['slots'][sched['taken']]
            sched['taken'] += 1
            return sl

        for _ in range(4):
            issue()
        self.norm_stage(self.xs, self.norm_ffn[layer], 'moe', layer)
        P.push()
        affT = P.sb("affT", [NEXP, S], F32)
        work = P.sb("rwork", [NEXP, S], F32)
        onesT = P.sb("ronesT", [NEXP, S], F32)
        m8 = P.sb("rm8", [NEXP, 8], F32)
        for b4 in range(4):
            bi = self.bank()
            for j in range(4):
                tt = b4 * 4 + j
                P.op('pe', lambda e, tt=tt, j=j: e.transpose(out=self.pb[bi][0:NEXP, j * 128:(j + 1) * 128], in_=self.aff[:, tt, :], identity=self.ident[:]),
                     reads=['aff', 'ident'], writes=['pb%d' % bi])
            P.op('dve', lambda e: e.tensor_copy(out=affT[:, b4 * 512:(b4 + 1) * 512], in_=self.pb[bi][0:NEXP, :]), reads=['pb%d' % bi], writes=['affT'])
        P.op('dve', lambda e: e.tensor_copy(out=work[:], in_=affT[:]), reads=['affT'], writes=['rwork'])
        P.op('dve', lambda e: e.memset(onesT[:], 1.0), writes=['ronesT'])
        for r in range(CAP // 8):
            P.op('dve', lambda e: e.max(out=m8[:], in_=work[:]), reads=['rwork'], writes=['rm8'])
            if r < CAP // 8 - 1:
                P.op('dve', lambda e: e.match_replace(out=work[:], in_to_replace=m8[:], in_values=work[:], imm_value=-1.0),
                     reads=['rm8', 'rwork'], writes=['rwork'])
        P.op('dve', lambda e: e.tensor_scalar(out=work[:], in0=affT[:], scalar1=m8[:, 7:8], scalar2=None, op0=ALU.is_ge),
             reads=['affT', 'rm8', 'rwork'], writes=['rwork'])
        P.op('dve', lambda e: e.tensor_tensor_scan(out=affT[:], data0=onesT[:], data1=work[:], initial=0.0, op0=ALU.mult, op1=ALU.add),
             reads=['ronesT', 'rwork', 'affT'], writes=['affT'])
        P.op('dve', lambda e: e.tensor_tensor(out=affT[:], in0=affT[:], in1=work[:], op=ALU.subtract), reads=['affT', 'rwork'], writes=['affT'])
        for (srcT, ksrc, dst, kdst) in ((work, 'rwork', mask_tm, 'mask_tm'), (affT, 'affT', pos_tm, 'pos_tm')):
            bi = self.bank()
            for tt in range(NT):
                P.op('pe', lambda e, tt=tt: e.transpose(out=self.pb[bi][:, tt * NEXP:(tt + 1) * NEXP], in_=srcT[:, tt * 128:(tt + 1) * 128],
                                                       identity=self.ident[0:NEXP, 0:NEXP]), reads=[ksrc, 'ident'], writes=['pb%d' % bi])
            P.op('dve', lambda e: e.tensor_copy(out=dst[:].rearrange("p t e -> p (t e)"), in_=self.pb[bi][:, 0:NT * NEXP]), reads=['pb%d' % bi], writes=[kdst])
        P.pop()
        P.op('dve', lambda e: e.tensor_copy(out=rg[:, :, :, 0], in_=tokidx[:, :].unsqueeze(2).to_broadcast([128, NT, NEXP])), reads=['tokidx'], writes=['rg'])
        P.op('dve', lambda e: e.tensor_copy(out=rg[:, :, :, 1], in_=self.aff[:]), reads=['aff', 'rg'], writes=['rg'])
        Sel = [P.sb("Sel%d" % i, [128, NT, CAP], F32) for i in range(2)]
        xe = [P.sb("xe%d" % i, [128, 2, D], F32) for i in range(2)]
        ye = [P.sb("ye%d" % i, [128, 2, D], F32) for i in range(2)]
        ig = [P.sb("ig%d" % i, [128, 4], F32) for i in range(2)]
        idx = [P.sb("idx%d" % i, [128, 2], I32) for i in range(2)]
        xeT = P.sb("xeT", [128, KC, CAP], BF16)
        hidT = P.sb("hidT", [128, KC, CAP], BF16)
        sg = [P.sb("sg%d" % i, [128, CAP], F32) for i in range(2)]

        def prep(e_):
            p2 = e_ % 2
            kS, kig, kidx, kxe = 'Sel%d' % p2, 'ig%d' % p2, 'idx%d' % p2, 'xe%d' % p2
            for tt in range(NT):
                P.op('dve', lambda e, tt=tt: e.tensor_scalar(out=Sel[p2][:, tt, :], in0=iota[:], scalar1=pos_tm[:, tt, e_:e_ + 1],
                                                            scalar2=mask_tm[:, tt, e_:e_ + 1], op0=ALU.is_equal, op1=ALU.mult),
                     reads=['iota', 'pos_tm', 'mask_tm'], writes=[kS])
            bi = self.bank()
            for jc in range(2):
                for tt in range(NT):
                    P.op('pe', lambda e, tt=tt, jc=jc: e.matmul(out=self.pb[bi][:, jc * 2:jc * 2 + 2], lhsT=Sel[p2][:, tt, jc * 128:(jc + 1) * 128],
                                                               rhs=rg[:, tt, e_, :], start=(tt == 0), stop=(tt == NT - 1)),
                         reads=[kS, 'rg'], writes=['pb%d' % bi])
            P.op('dve', lambda e: e.tensor_copy(out=ig[p2][:], in_=self.pb[bi][:, 0:4]), reads=['pb%d' % bi], writes=[kig])
            P.op('dve', lambda e: e.tensor_copy(out=idx[p2][:], in_=ig[p2][:, 0:4:2]), reads=[kig], writes=[kidx])
            for jc in range(2):
                P.dma('pool', lambda g, jc=jc: g.indirect_dma_start(out=xe[p2][:, jc, :], out_offset=None, in_=self.h2d,
                                                                   in_offset=bass.IndirectOffsetOnAxis(ap=idx[p2][:, jc:jc + 1], axis=0)),
                      reads=[kidx, 'h2d'], writes=[kxe])

        prep(0)
        for ex_ in range(NEXP):
            p2 = ex_ % 2
            kig, kidx, kxe, kye = 'ig%d' % p2, 'idx%d' % p2, 'xe%d' % p2, 'ye%d' % p2
            for k2 in range(KC // 2):
                bi = self.bank()
                for q in range(4):
                    kc = k2 * 2 + q // 2
                    jc = q % 2
                    P.op('pe', lambda e, kc=kc, jc=jc, q=q: e.transpose(out=self.pb[bi][:, q * 128:(q + 1) * 128], in_=xe[p2][:, jc, kc * 128:(kc + 1) * 128],
                                                                       identity=self.ident[:]), reads=[kxe, 'ident'], writes=['pb%d' % bi])
                dst = xeT[:, k2 * 2:k2 * 2 + 2, :].rearrange("p a b -> p (a b)")
                if k2 % 2 == 0:
                    P.op('act', lambda e: e.copy(out=dst, in_=self.pb[bi][:]), reads=['pb%d' % bi], writes=['xeT'])
                else:
                    P.op('dve', lambda e: e.tensor_copy(out=dst, in_=self.pb[bi][:]), reads=['pb%d' % bi], writes=['xeT'])
            for fs in range(4):
                sl_g = take()
                sl_u = take()
                for fc in range(4):
                    bi = self.bank()
                    for (off, slot) in ((0, sl_g), (256, sl_u)):
                        for kc in range(KC):
                            P.op('pe', lambda e, kc=kc, off=off, slot=slot: e.matmul(out=self.pb[bi][:, off:off + 256], lhsT=self.ring[slot][:, kc, fc * 128:(fc + 1) * 128],
                                                                                    rhs=xeT[:, kc, :], start=(kc == 0), stop=(kc == KC - 1)),
                                 reads=['xeT', 'ring%d' % slot], writes=['pb%d' % bi])
                    s_ = sg[(fs * 4 + fc) % 2]
                    ks = 'sg%d' % ((fs * 4 + fc) % 2)
                    P.op('act', lambda e: e.activation(out=s_[:], in_=self.pb[bi][:, 0:256], func=AF.Silu), reads=['pb%d' % bi], writes=[ks])
                    P.op('dve', lambda e: e.tensor_tensor(out=hidT[:, fs * 4 + fc, :], in0=s_[:], in1=self.pb[bi][:, 256:512], op=ALU.mult),
                         reads=[ks, 'pb%d' % bi], writes=['hidT'])
                issue()
                issue()
            if ex_ + 1 < NEXP:
                prep(ex_ + 1)
            for ds_ in range(4):
                sl_d = take()
                for jc in range(2):
                    bi = self.bank()
                    for kc in range(KC):
                        P.op('pe', lambda e, kc=kc: e.matmul(out=self.pb[bi][:], lhsT=hidT[:, kc, jc * 128:(jc + 1) * 128], rhs=self.ring[sl_d][:, kc, :],
                                                            start=(kc == 0), stop=(kc == KC - 1)),
                             reads=['hidT', 'ring%d' % sl_d], writes=['pb%d' % bi])
                    dsty = ye[p2][:, jc, ds_ * 512:(ds_ + 1) * 512]
                    gsc = ig[p2][:, 2 * jc + 1:2 * jc + 2]
                    if jc == 0:
                        P.op('act', lambda e: e.activation(out=dsty, in_=self.pb[bi][:], func=AF.Copy, scale=gsc), reads=['pb%d' % bi, kig], writes=[kye])
                    else:
                        P.op('dve', lambda e: e.tensor_scalar(out=dsty, in0=self.pb[bi][:], scalar1=gsc, scalar2=None, op0=ALU.mult),
                             reads=['pb%d' % bi, kig], writes=[kye])
                issue()
            for jc in range(2):
                P.dma('pool', lambda g, jc=jc: g.indirect_dma_start(out=self.xs, out_offset=bass.IndirectOffsetOnAxis(ap=idx[p2][:, jc:jc + 1], axis=0),
                                                                   in_=ye[p2][:, jc, :], in_offset=None, compute_op=ALU.add),
                      reads=[kye, kidx, 'xs_w'], writes=['xs'])
        P.pop()


def _prep_inputs(inputs):
    f = lambda a: np.ascontiguousarray(np.asarray(a, dtype=np.float32))
    shared = {
        'norm_mix': f(inputs['norm_mix']), 'norm_ffn': f(inputs['norm_ffn']), 'norm_final': f(inputs['norm_final']),
        'w_in_ab': f(inputs['w_in_ab'][0]), 'a_v_norm': f(inputs['a_v_norm'][0]),
        'a_spatial_wT': f(np.transpose(np.asarray(inputs['a_spatial_w'][0]), (2, 0, 1))),
        'a_spatial_bT': f(np.transpose(np.asarray(inputs['a_spatial_b'][0]), (1, 0))),
        'na_tab': na_bias_table(np.asarray(inputs['b_rpb'][0], dtype=np.float32)),
        'w_out_ab': f(inputs['w_out_ab'][0]), 'w_in_cd': f(inputs['w_in_cd'][0]),
        'c_convT': f(np.transpose(np.asarray(inputs['c_conv'][0]).reshape(3, 8, 128), (2, 1, 0))),
        'dil_tab': dil_bias_table(),
        'w_out_cd': f(inputs['w_out_cd'][0]), 'router': f(inputs['router']),
        'w_gate': f(inputs['w_gate']), 'w_up': f(inputs['w_up']), 'w_down': f(inputs['w_down']),
        'ident': np.eye(128, dtype=np.float32),
        'iota': np.tile(np.arange(256, dtype=np.float32)[None, :], (128, 1)),
        'tokidx': f(np.arange(128)[:, None] + 128 * np.arange(NT)[None, :]),
    }
    return shared


def run(inputs, stop=None, trace=False, ncores=8, start=0):
    mk = MK(stop=stop, start=start)
    shared = _prep_inputs(inputs)
    x = np.asarray(inputs['x'], dtype=np.float32)
    in_maps = []
    for c in range(ncores):
        m = {k: v for k, v in shared.items() if k in mk.declared}
        m['x'] = np.ascontiguousarray(x[c])
        in_maps.append(m)
    res = run_bass_kernel_spmd(mk.nc, in_maps, core_ids=list(range(ncores)), trace=trace)
    out = np.stack([res.results[c]['out'] for c in range(ncores)], axis=0)
    return out, res


def kernel(**inputs):
    out, _ = run(inputs)
    return out.astype(np.float32)
```

```python
import numpy as np
from contextlib import ExitStack
import concourse.bass as bass
import concourse.mybir as mybir
from concourse.bass_utils import run_bass_kernel_spmd

F32 = mybir.dt.float32
BF16 = mybir.dt.bfloat16
I32 = mybir.dt.int32
AF = mybir.ActivationFunctionType
ALU = mybir.AluOpType
AX = mybir.AxisListType

SEM_EPOCH = 30000


class Prog:
    def __init__(self, nc, n_dma_sems=6):
        self.nc = nc
        self.nsem = 0
        self.stack = ExitStack()
        self.root = ExitStack()
        self.scopes = []
        self.eng = {'pe': nc.tensor, 'act': nc.scalar, 'dve': nc.vector,
                    'pool': nc.gpsimd, 'sp': nc.sync}
        self.sem = {}
        self.cnt = {}
        for k in ['pe', 'act', 'dve', 'pool']:
            self.sem[k] = self._newsem("s_" + k)
            self.cnt[k] = 0
        self.waited = {e: {} for e in self.eng}
        self.last_w = {}
        self.readers = {}
        self.dsem = {}
        self.dsem_i = {}
        self.n_dma_sems = n_dma_sems

    def _newsem(self, name):
        self.nsem = getattr(self, 'nsem', 0) + 1
        return self.root.enter_context(self.nc.semaphore(name + "_%d" % self.nsem))

    def sb(self, name, shape, dtype):
        self.nsem += 1
        return self.stack.enter_context(self.nc.sbuf_tensor("sb_%s_%d" % (name, self.nsem), shape, dtype))

    def ps(self, name, shape, dtype=F32):
        self.nsem += 1
        return self.stack.enter_context(self.nc.psum_tensor("ps_%s_%d" % (name, self.nsem), shape, dtype))

    def _deps(self, reads, writes):
        deps = []
        for r in reads:
            if r in self.last_w:
                deps.append(self.last_w[r])
        for w in writes:
            if w in self.last_w:
                deps.append(self.last_w[w])
            deps.extend(self.readers.get(w, ()))
        return deps

    def _emit_waits(self, engine, deps):
        e = self.eng[engine]
        wd = self.waited[engine]
        best = {}
        for (sem, val, src) in deps:
            if src == 'pe' and engine == 'pe':
                continue
            k = id(sem)
            if wd.get(k, 0) >= val:
                continue
            if k not in best or best[k][1] < val:
                best[k] = (sem, val)
        for k, (sem, val) in best.items():
            e.wait_ge(sem, val)
            wd[k] = val

    def _record(self, ev, reads, writes):
        for w in writes:
            self.last_w[w] = ev
            self.readers[w] = []
        for r in reads:
            if r not in writes:
                self.readers.setdefault(r, []).append(ev)

    def op(self, engine, fn, reads=(), writes=()):
        deps = self._deps(reads, writes)
        self._emit_waits(engine, deps)
        ins = fn(self.eng[engine])
        if self.cnt[engine] >= SEM_EPOCH:
            self.sem[engine] = self._newsem("s_" + engine)
            self.cnt[engine] = 0
        self.cnt[engine] += 1
        ins.then_inc(self.sem[engine], 1)
        ev = (self.sem[engine], self.cnt[engine], engine)
        self._record(ev, reads, writes)
        return ev

    def dma(self, queue, out, in_=None, reads=(), writes=(), **kw):
        deps = self._deps(reads, writes)
        if queue not in self.dsem:
            self.dsem[queue] = [[self._newsem("d_" + queue), 0] for _ in range(self.n_dma_sems)]
            self.dsem_i[queue] = 0
        i = self.dsem_i[queue]
        self.dsem_i[queue] = (i + 1) % self.n_dma_sems
        slot = self.dsem[queue][i]
        if slot[1] >= SEM_EPOCH:
            deps.append((slot[0], slot[1], None))
            slot[0] = self._newsem("d_" + queue)
            slot[1] = 0
        if slot[1] > 0:
            deps.append((slot[0], slot[1], None))
        self._emit_waits(queue, deps)
        if callable(out):
            ins = out(self.eng[queue])
        else:
            ins = self.eng[queue].dma_start(out=out, in_=in_, **kw)
        slot[1] += 16
        ins.then_inc(slot[0], 16)
        ev = (slot[0], slot[1], None)
        self._record(ev, reads, writes)
        return ev

    def barrier(self):
        evs = [(self.sem[e], self.cnt[e], e) for e in self.sem if self.cnt[e] > 0]
        for q in self.dsem:
            for slot in self.dsem[q]:
                if slot[1] > 0:
                    evs.append((slot[0], slot[1], None))
        for x in self.eng:
            e = self.eng[x]
            wd = self.waited[x]
            for (sem, val, src) in evs:
                if wd.get(id(sem), 0) >= val:
                    continue
                e.wait_ge(sem, val)
                wd[id(sem)] = val
        self.last_w = {}
        self.readers = {}

    def push(self):
        self.scopes.append(self.stack)
        self.stack = ExitStack()

    def pop(self):
        self.barrier()
        self.stack.close()
        self.stack = self.scopes.pop()

    def finish(self, out_keys, engine='sp'):
        deps = [self.last_w[k] for k in out_keys]
        self._emit_waits(engine, deps)
        self.stack.close()
        self.root.close()


S = 2048
D = 2048
NT = 16
KC = 16
EPS_RMS = 1e-6
EPS_LN = 1e-5
NEG = -30000.0
NEXP = 16
CAP = 256


def _na_chunks(m):
    rows = S // 64
    rs = lambda r: int(np.clip(r - 4, 0, rows - 8))
    lo = rs(2 * m) // 2
    hi = (rs(2 * m + 1) + 7) // 2
    return list(range(lo, hi + 1))


def _na_tile_index():
    idx = {}
    n = 0
    for rel in range(-2, 3):
        idx[('mid', rel)] = n
        n += 1
    for m in (0, 1, 14, 15):
        for j in _na_chunks(m):
            idx[(m, j)] = n
            n += 1
    return idx, n


def _na_tile_id(idx, m, j):
    if 2 <= m <= 13:
        return idx[('mid', j - m)]
    return idx[(m, j)]


def na_bias_table(rpb):
    idx, n = _na_tile_index()
    H = rpb.shape[0]
    rows = S // 64
    tab = np.full((H, 128, n, 128), NEG, np.float32)
    done = set()
    for m in range(16):
        for j in _na_chunks(m):
            t = _na_tile_id(idx, m, j)
            if t in done:
                continue
            done.add(t)
            kk = np.arange(128)
            qq = np.arange(128)
            krow = 2 * j + kk // 64
            kcol = kk % 64
            qrow = 2 * m + qq // 64
            qcol = qq % 64
            rs = np.clip(qrow - 4, 0, rows - 8)
            cs = np.clip(qcol - 8, 0, 64 - 16)
            valid = ((krow[:, None] >= rs[None, :]) & (krow[:, None] < rs[None, :] + 8) &
                     (kcol[:, None] >= cs[None, :]) & (kcol[:, None] < cs[None, :] + 16))
            dr = np.clip(krow[:, None] - qrow[None, :] + 7, 0, 14)
            dc = np.clip(kcol[:, None] - qcol[None, :] + 15, 0, 30)
            g = rpb[:, dr, dc]
            tab[:, :, t, :] = np.where(valid[None], g, np.float32(NEG))
    return tab


def dil_bias_table():
    H = 8
    slopes = np.array([2.0 ** (-8.0 * (h + 1) / H) for h in range(H)], np.float64)
    tab = np.full((H, 128, 17, 128), NEG, np.float32)
    kk = np.arange(128)[:, None]
    qq = np.arange(128)[None, :]
    for rel in range(-8, 9):
        delta = rel * 128 + kk - qq
        ad = np.abs(delta)
        mult = np.zeros_like(ad)
        for (w, dil) in ((128, 1), (512, 4), (2048, 16)):
            rad = w // (2 * dil)
            mult += ((ad % dil == 0) & (ad // dil <= rad)).astype(ad.dtype)
        for h in range(H):
            v = np.where(mult > 0, np.log(np.maximum(mult, 1)) - slopes[h] * ad, NEG)
            tab[h, :, rel + 8, :] = v.astype(np.float32)
    return tab


class MK:
    def __init__(self, nlayers=2, stop=None, debug=False, start=0):
        self.stop = stop
        self.start = start
        nc = bass.Bass("TRN2", target_bir_lowering=False)
        self.nc = nc
        self.P = Prog(nc)
        P = self.P
        self.declared = []

        def di(name, shape, dt=F32):
            self.declared.append(name)
            return nc.dram_tensor(name, shape, dt, kind="ExternalInput").ap()
        ds = lambda name, shape, dt=F32: nc.dram_tensor(name, shape, dt, kind="Internal").ap()
        self.x_in = di("x", [S, D])
        self.norm_mix = di("norm_mix", [2, D])
        self.norm_ffn = di("norm_ffn", [2, D])
        self.norm_final = di("norm_final", [D])
        self.w_in_ab = di("w_in_ab", [D, 5120])
        self.lng = di("a_v_norm", [1024])
        self.wsT = di("a_spatial_wT", [128, 8, 128])
        self.bsT = di("a_spatial_bT", [128, 8])
        self.na_tab = di("na_tab", [8, 128, 21, 128])
        self.w_out_ab = di("w_out_ab", [D, D])
        self.w_in_cd = di("w_in_cd", [D, 6144])
        self.taps = di("c_convT", [128, 8, 3])
        self.dil_tab = di("dil_tab", [8, 128, 17, 128])
        self.w_out_cd = di("w_out_cd", [D, D])
        self.router = di("router", [2, D, NEXP])
        if stop not in ('mix0', 'mix1only'):
            self.w_gate = di("w_gate", [2, NEXP, D, D])
            self.w_up = di("w_up", [2, NEXP, D, D])
            self.w_down = di("w_down", [2, NEXP, D, D])
        self.ident_in = di("ident", [128, 128])
        self.iota_in = di("iota", [128, 256])
        self.tokidx_in = di("tokidx", [128, NT])
        self.out = nc.dram_tensor("out", [S, D], F32, kind="ExternalOutput").ap()
        self.xs = ds("xs", [S, D])
        self.h2d = ds("h2d", [S, D])
        self.yTd = ds("yTd", [NT, 128, 8, 128], BF16)
        self.QTd = ds("QTd", [8, 128, S], BF16)
        self.KTd = ds("KTd", [8, 128, S], BF16)
        self.Vd = ds("Vd", [S, 1024], BF16)
        self.ring = [P.sb("ring%d" % i, [128, KC, 512], BF16) for i in range(4)]
        self.ring_n = 0
        self.ident = P.sb("ident", [128, 128], F32)
        self.ones_bf = P.sb("ones_bf", [128, 128], BF16)
        self.pb = [P.ps("pb%d" % i, [128, 512], F32) for i in range(8)]
        self.pb_n = 0
        P.dma('sp', self.ident[:], self.ident_in, writes=['ident'])
        P.op('dve', lambda e: e.memset(self.ones_bf[:], 1.0), writes=['ones_bf'])

        if start == 0:
            self.layer_mix(0)
            if stop == 'mix0':
                return self.finish(self.xs)
            self.moe(0)
            if stop == 'moe0':
                return self.finish(self.xs)
        self.layer_mix(1)
        if stop in ('mix1', 'mix1only'):
            return self.finish(self.xs)
        self.moe(1)
        if stop == 'moe1':
            return self.finish(self.xs)
        self.norm_stage(self.xs, self.norm_final, 'final')
        P.finish([])

    def finish(self, src):
        P = self.P
        P.push()
        t = P.sb("cp", [128, D], F32)
        for tt in range(NT):
            P.dma('sp', t[:], src[tt * 128:(tt + 1) * 128, :], reads=['xs'], writes=['cp'])
            P.dma('sp', self.out[tt * 128:(tt + 1) * 128, :], t[:], reads=['cp'], writes=['out'])
        P.pop()
        P.finish([])

    def bank(self):
        i = self.pb_n % 8
        self.pb_n += 1
        return i

    def load_slab(self, w_ap, col0):
        i = self.ring_n % 4
        self.ring_n += 1
        src = w_ap[:, col0:col0 + 512].rearrange("(kc p) f -> p kc f", p=128)
        self.P.dma('pool', self.ring[i][:], src, writes=['ring%d' % i])
        return i

    def norm_stage(self, src, gvec, mode, layer=0):
        P = self.P
        P.push()
        gb = P.sb("gb", [128, D], F32)
        P.dma('sp', gb[:], gvec.partition_broadcast(128), writes=['gb'])
        xb = [P.sb("xb%d" % i, [128, D], F32) for i in range(2)]
        hb = [P.sb("hb%d" % i, [128, D], F32) for i in range(2)]
        ss = P.sb("ss", [128, 4], F32)
        if mode == 'moe':
            rt = P.sb("rt", [128, KC, NEXP], F32)
            P.dma('sp', rt[:], self.router[layer].rearrange("(kc p) e -> p kc e", p=128), writes=['rt'])
            hT32 = P.sb("hT32", [128, KC, 128], F32)
            sm = P.sb("sm", [128, 8], F32)
            ex = P.sb("ex", [128, NEXP], F32)
        for tt in range(NT):
            x_t = xb[tt % 2]
            h_t = hb[tt % 2]
            kx, kh = 'xb%d' % (tt % 2), 'hb%d' % (tt % 2)
            P.dma('sp', x_t[:], src[tt * 128:(tt + 1) * 128, :], reads=['xs'], writes=[kx])
            P.op('act', lambda e: e.activation(out=h_t[:], in_=x_t[:], func=AF.Square, accum_out=ss[:, 0:1]),
                 reads=[kx], writes=[kh, 'ss'])
            P.op('act', lambda e: e.activation(out=ss[:, 1:2], in_=ss[:, 0:1], func=AF.Sqrt, scale=1.0 / D, bias=EPS_RMS),
                 reads=['ss'], writes=['ss'])
            P.op('dve', lambda e: e.reciprocal(out=ss[:, 2:3], in_=ss[:, 1:2]), reads=['ss'], writes=['ss2'])
            P.op('dve', lambda e: e.scalar_tensor_tensor(out=h_t[:], in0=x_t[:], scalar=ss[:, 2:3], in1=gb[:],
                                                         op0=ALU.mult, op1=ALU.mult),
                 reads=[kx, 'ss2', 'gb', kh], writes=[kh])
            if mode == 'final':
                P.dma('sp', self.out[tt * 128:(tt + 1) * 128, :], h_t[:], reads=[kh], writes=['out'])
                continue
            if mode == 'moe':
                P.dma('sp', self.h2d[tt * 128:(tt + 1) * 128, :], h_t[:], reads=[kh], writes=['h2d'])
            for b4 in range(4):
                bi = self.bank()
                kb = 'pb%d' % bi
                for j in range(4):
                    kc = b4 * 4 + j
                    P.op('pe', lambda e, kc=kc, j=j: e.transpose(out=self.pb[bi][:, j * 128:(j + 1) * 128],
                                                                in_=h_t[:, kc * 128:(kc + 1) * 128],
                                                                identity=self.ident[:]),
                         reads=[kh, 'ident'], writes=[kb])
                if mode == 'mix':
                    dst = self.hT[:, b4 * 4:(b4 + 1) * 4, tt * 128:(tt + 1) * 128]
                    wk = 'hT'
                else:
                    dst = hT32[:, b4 * 4:(b4 + 1) * 4, :]
                    wk = 'hT32'
                srcp = self.pb[bi][:].rearrange("p (j t) -> p j t", j=4)
                if b4 % 2 == 0:
                    P.op('act', lambda e: e.copy(out=dst, in_=srcp), reads=[kb], writes=[wk])
                else:
                    P.op('dve', lambda e: e.tensor_copy(out=dst, in_=srcp), reads=[kb], writes=[wk])
            if mode == 'moe':
                bi = self.bank()
                kb = 'pb%d' % bi
                for kc in range(KC):
                    P.op('pe', lambda e, kc=kc: e.matmul(out=self.pb[bi][:, 0:NEXP], lhsT=hT32[:, kc, :], rhs=rt[:, kc, :],
                                                        start=(kc == 0), stop=(kc == KC - 1)),
                         reads=['hT32', 'rt'], writes=[kb])
                lg = self.pb[bi][:, 0:NEXP]
                P.op('dve', lambda e: e.tensor_reduce(out=sm[:, 0:1], in_=lg, op=ALU.max, axis=AX.X, negate=True),
                     reads=[kb], writes=['sm'])
                P.op('act', lambda e: e.activation(out=ex[:], in_=lg, func=AF.Exp, bias=sm[:, 0:1], accum_out=sm[:, 1:2]),
                     reads=[kb, 'sm'], writes=['ex', 'sm'])
                P.op('dve', lambda e: e.reciprocal(out=sm[:, 2:3], in_=sm[:, 1:2]), reads=['sm'], writes=['sm2'])
                P.op('dve', lambda e: e.tensor_scalar(out=self.aff[:, tt, :], in0=ex[:], scalar1=sm[:, 2:3], scalar2=None,
                                                      op0=ALU.mult),
                     reads=['ex', 'sm2'], writes=['aff'])
        P.pop()

    def layer_mix(self, layer):
        P = self.P
        src = self.x_in if layer == self.start else self.xs
        w_in = self.w_in_ab if layer == 0 else self.w_in_cd
        w_out = self.w_out_ab if layer == 0 else self.w_out_cd
        P.push()
        if layer == 1:
            self.yaT = P.sb("yaT", [128, 8, S], BF16)
        P.push()
        self.hT = P.sb("hT", [128, KC, S], BF16)
        if layer == 0:
            pre = [self.load_slab(w_in, c * 512) for c in range(4)]
        else:
            pre = [self.load_slab(w_in, 1024), self.load_slab(w_in, 2048), self.load_slab(w_in, 0)]
        self.norm_stage(src, self.norm_mix[layer], 'mix')
        if layer == 0:
            self.sgu_stage(w_in, pre)
            qkv0 = 2048
        else:
            self.conv_stage(w_in, pre)
            qkv0 = 3072
        self.qkv_stage(w_in, qkv0)
        P.pop()
        self.ybT = P.sb("ybT", [128, 8, S], BF16)
        wslots = [self.load_slab(w_out, db * 512) for db in range(4)]
        self.attn_stage(layer)
        self.outproj_stage(layer, src, wslots)
        P.pop()

    def gelu_tanh(self, pbank, kb, dst, kdst, tmp, ktmp):
        P = self.P
        P.op('act', lambda e: e.activation(out=tmp, in_=pbank, func=AF.Square), reads=[kb], writes=[ktmp])
        P.op('dve', lambda e: e.tensor_scalar(out=tmp, in0=tmp, scalar1=0.044715, scalar2=1.0, op0=ALU.mult, op1=ALU.add),
             reads=[ktmp], writes=[ktmp])
        P.op('dve', lambda e: e.tensor_tensor(out=tmp, in0=tmp, in1=pbank, op=ALU.mult), reads=[ktmp, kb], writes=[ktmp])
        P.op('act', lambda e: e.activation(out=tmp, in_=tmp, func=AF.Sigmoid, scale=1.5957691216057308),
             reads=[ktmp], writes=[ktmp])
        P.op('dve', lambda e: e.tensor_tensor(out=dst, in0=tmp, in1=pbank, op=ALU.mult), reads=[ktmp, kb], writes=[kdst])

    def sgu_stage(self, w_in, slots):
        P = self.P
        P.push()
        lngb = P.sb("lngb", [128, 1024], F32)
        P.dma('sp', lngb[:], self.lng.partition_broadcast(128), writes=['lngb'])
        wsT = P.sb("wsT", [128, 8, 128], BF16)
        P.dma('pool', wsT[:], self.wsT, writes=['wsT'])
        bsT = P.sb("bsT", [128, 8], F32)
        P.dma('sp', bsT[:], self.bsT, writes=['bsT'])
        ut = P.sb("ut", [128, 1024], F32)
        vf = P.sb("vf", [128, 1024], F32)
        vn = P.sb("vn", [128, 1024], BF16)
        tmp = [P.sb("gtmp%d" % i, [128, 512], F32) for i in range(2)]
        ya = P.sb("ya", [128, 1024], F32)
        st = P.sb("bnst", [128, 2, 6], F32)
        mv = P.sb("bnmv", [128, 4], F32)
        yst = [P.sb("yst%d" % i, [128, 8, 128], BF16) for i in range(2)]
        for tt in range(NT):
            banks = []
            for s4 in range(4):
                bi = self.bank()
                banks.append(bi)
                for kc in range(KC):
                    P.op('pe', lambda e, kc=kc: e.matmul(out=self.pb[bi][:], lhsT=self.hT[:, kc, tt * 128:(tt + 1) * 128],
                                                        rhs=self.ring[slots[s4]][:, kc, :], start=(kc == 0), stop=(kc == KC - 1)),
                         reads=['hT', 'ring%d' % slots[s4]], writes=['pb%d' % bi])
            for s4 in range(4):
                bi = banks[s4]
                dstt, kd = (ut, 'ut') if s4 < 2 else (vf, 'vf')
                c0 = (s4 % 2) * 512
                self.gelu_tanh(self.pb[bi][:], 'pb%d' % bi, dstt[:, c0:c0 + 512], kd, tmp[s4 % 2][:], 'gtmp%d' % (s4 % 2))
            for hh in range(2):
                P.op('dve', lambda e, hh=hh: e.bn_stats(out=st[:, hh, :], in_=vf[:, hh * 512:(hh + 1) * 512]),
                     reads=['vf'], writes=['bnst'])
            P.op('dve', lambda e: e.bn_aggr(out=mv[:, 0:2], in_=st[:].rearrange("p a b -> p (a b)")), reads=['bnst'], writes=['bnmv'])
            P.op('act', lambda e: e.activation(out=mv[:, 2:3], in_=mv[:, 1:2], func=AF.Sqrt, bias=EPS_LN), reads=['bnmv'], writes=['bnmv2'])
            P.op('dve', lambda e: e.reciprocal(out=mv[:, 3:4], in_=mv[:, 2:3]), reads=['bnmv2'], writes=['bnmv3'])
            P.op('dve', lambda e: e.tensor_scalar(out=vf[:], in0=vf[:], scalar1=mv[:, 0:1], scalar2=mv[:, 3:4],
                                                  op0=ALU.subtract, op1=ALU.mult), reads=['vf', 'bnmv', 'bnmv3'], writes=['vf'])
            P.op('dve', lambda e: e.tensor_tensor(out=vn[:], in0=vf[:], in1=lngb[:], op=ALU.mult), reads=['vf', 'lngb'], writes=['vn'])
            sb_ = [self.bank(), self.bank()]
            for g in range(8):
                bi = sb_[g // 4]
                P.op('pe', lambda e, g=g: e.matmul(out=self.pb[bi][:, (g % 4) * 128:(g % 4 + 1) * 128], lhsT=wsT[:, g, :],
                                                   rhs=vn[:, g * 128:(g + 1) * 128], start=True, stop=True),
                     reads=['wsT', 'vn'], writes=['pb%d' % bi])
            for hh in range(2):
                bi = sb_[hh]
                yv = ya[:, hh * 512:(hh + 1) * 512].rearrange("p (g c) -> p g c", g=4)
                P.op('dve', lambda e: e.tensor_tensor(out=yv, in0=self.pb[bi][:].rearrange("p (g c) -> p g c", g=4),
                                                      in1=bsT[:, hh * 4:(hh + 1) * 4].unsqueeze(2).to_broadcast([128, 4, 128]), op=ALU.add),
                     reads=['pb%d' % bi, 'bsT'], writes=['ya'])
            P.op('dve', lambda e: e.tensor_tensor(out=ya[:], in0=ya[:], in1=ut[:], op=ALU.mult), reads=['ya', 'ut'], writes=['ya'])
            ys = yst[tt % 2]
            ky = 'yst%d' % (tt % 2)
            for hh in range(2):
                bi = self.bank()
                for j in range(4):
                    c = hh * 4 + j
                    P.op('pe', lambda e, c=c, j=j: e.transpose(out=self.pb[bi][:, j * 128:(j + 1) * 128], in_=ya[:, c * 128:(c + 1) * 128],
                                                              identity=self.ident[:]), reads=['ya', 'ident'], writes=['pb%d' % bi])
                P.op('act', lambda e: e.copy(out=ys[:, hh * 4:(hh + 1) * 4, :], in_=self.pb[bi][:].rearrange("p (j t) -> p j t", j=4)),
                     reads=['pb%d' % bi], writes=[ky])
            P.dma('sp', self.yTd[tt], ys[:], reads=[ky], writes=['yTd'])
        P.pop()

    def qkv_stage(self, w_in, col0):
        P = self.P
        P.push()
        stg = [P.sb("qstg%d" % i, [128, S], BF16) for i in range(2)]
        vst = [P.sb("vstg%d" % i, [128, 512], BF16) for i in range(2)]
        n = 0
        for which in range(2):
            dstd = self.QTd if which == 0 else self.KTd
            scale = (128.0 ** -0.5) if which == 0 else 1.0
            for sl in range(2):
                slot = self.load_slab(w_in, col0 + which * 1024 + sl * 512)
                for fc in range(4):
                    h = sl * 4 + fc
                    sg = stg[n % 2]
                    ks = 'qstg%d' % (n % 2)
                    n += 1
                    for tb in range(4):
                        bi = self.bank()
                        for kc in range(KC):
                            P.op('pe', lambda e, kc=kc: e.matmul(out=self.pb[bi][:], lhsT=self.ring[slot][:, kc, fc * 128:(fc + 1) * 128],
                                                                rhs=self.hT[:, kc, tb * 512:(tb + 1) * 512], start=(kc == 0), stop=(kc == KC - 1)),
                                 reads=['hT', 'ring%d' % slot], writes=['pb%d' % bi])
                        if tb % 2 == 0:
                            P.op('act', lambda e: e.activation(out=sg[:, tb * 512:(tb + 1) * 512], in_=self.pb[bi][:], func=AF.Copy, scale=scale),
                                 reads=['pb%d' % bi], writes=[ks])
                        else:
                            P.op('dve', lambda e: e.tensor_scalar(out=sg[:, tb * 512:(tb + 1) * 512], in0=self.pb[bi][:], scalar1=scale, scalar2=None, op0=ALU.mult),
                                 reads=['pb%d' % bi], writes=[ks])
                    P.dma('sp', dstd[h], sg[:], reads=[ks], writes=['QKd'])
        m = 0
        for sl in range(2):
            slot = self.load_slab(w_in, col0 + 2048 + sl * 512)
            for tt in range(NT):
                bi = self.bank()
                for kc in range(KC):
                    P.op('pe', lambda e, kc=kc: e.matmul(out=self.pb[bi][:], lhsT=self.hT[:, kc, tt * 128:(tt + 1) * 128],
                                                        rhs=self.ring[slot][:, kc, :], start=(kc == 0), stop=(kc == KC - 1)),
                         reads=['hT', 'ring%d' % slot], writes=['pb%d' % bi])
                vs = vst[m % 2]
                kv = 'vstg%d' % (m % 2)
                m += 1
                if tt % 2 == 0:
                    P.op('act', lambda e: e.copy(out=vs[:], in_=self.pb[bi][:]), reads=['pb%d' % bi], writes=[kv])
                else:
                    P.op('dve', lambda e: e.tensor_copy(out=vs[:], in_=self.pb[bi][:]), reads=['pb%d' % bi], writes=[kv])
                P.dma('sp', self.Vd[tt * 128:(tt + 1) * 128, sl * 512:(sl + 1) * 512], vs[:], reads=[kv], writes=['Vd'])
        P.pop()

    def attn_stage(self, layer):
        P = self.P
        P.push()
        if layer == 0:
            idx, ntile = _na_tile_index()
            tab = self.na_tab
            chunks = lambda i: [(j, _na_tile_id(idx, i, j)) for j in _na_chunks(i)]
        else:
            ntile = 17
            tab = self.dil_tab
            chunks = lambda i: [(j, j - i + 8) for j in range(max(0, i - 8), min(15, i + 8) + 1)]
        QT = [P.sb("aQT%d" % i, [128, S], BF16) for i in range(2)]
        KT = [P.sb("aKT%d" % i, [128, S], BF16) for i in range(2)]
        V = [P.sb("aV%d" % i, [128, NT, 128], BF16) for i in range(2)]
        Tb = [P.sb("aTb%d" % i, [128, ntile, 128], F32) for i in range(2)]
        E = [P.sb("aE%d" % i, [128, ntile, 128], BF16) for i in range(2)]
        pexp = [P.sb("apexp%d" % i, [128, 4, 128], BF16) for i in range(2)]
        pt = [P.sb("apt%d" % i, [128, 4, 128], BF16) for i in range(3)]
        rden = [P.sb("arden%d" % i, [128, 128], F32) for i in range(2)]
        nst = 0
        nq = 0
        nb_ = 0
        for h in range(8):
            hp = h % 2
            kq, kk, kv, kt, ke = 'aQT%d' % hp, 'aKT%d' % hp, 'aV%d' % hp, 'aTb%d' % hp, 'aE%d' % hp
            P.dma('sp', QT[hp][:], self.QTd[h], reads=['QKd'], writes=[kq])
            P.dma('sp', KT[hp][:], self.KTd[h], reads=['QKd'], writes=[kk])
            P.dma('sp', V[hp][:], self.Vd[:, h * 128:(h + 1) * 128].rearrange("(t p) c -> p t c", p=128), reads=['Vd'], writes=[kv])
            P.dma('sp', Tb[hp][:], tab[h], writes=[kt])
            P.op('act', lambda e: e.activation(out=E[hp][:], in_=Tb[hp][:], func=AF.Exp), reads=[kt], writes=[ke])
            for i in range(NT):
                cl = chunks(i)
                bo = 4 + (nq % 2)
                bd = 6 + (nq % 2)
                rd = rden[nq % 2]
                krd = 'arden%d' % (nq % 2)
                nq += 1
                ncl = len(cl)
                done = 0
                for b0 in range(0, ncl, 4):
                    batch = cl[b0:b0 + 4]
                    nb = len(batch)
                    bi = nst % 4
                    nst += 1
                    kb = 'pb%d' % bi
                    for jj, (j, tid) in enumerate(batch):
                        P.op('pe', lambda e, jj=jj, j=j: e.matmul(out=self.pb[bi][:, jj * 128:(jj + 1) * 128], lhsT=KT[hp][:, j * 128:(j + 1) * 128],
                                                                  rhs=QT[hp][:, i * 128:(i + 1) * 128], start=True, stop=True),
                             reads=[kq, kk], writes=[kb])
                    pe_ = pexp[nb_ % 2]
                    kpe = 'apexp%d' % (nb_ % 2)
                    p_t = pt[nb_ % 3]
                    kpt = 'apt%d' % (nb_ % 3)
                    nb_ += 1
                    P.op('act', lambda e: e.activation(out=pe_[:, 0:nb, :], in_=self.pb[bi][:, 0:nb * 128].rearrange("p (j t) -> p j t", j=nb), func=AF.Exp),
                         reads=[kb], writes=[kpe])
                    t0 = batch[0][1]
                    P.op('dve', lambda e: e.tensor_tensor(out=p_t[:, 0:nb, :], in0=pe_[:, 0:nb, :], in1=E[hp][:, t0:t0 + nb, :], op=ALU.mult),
                         reads=[kpe, ke], writes=[kpt])
                    for jj, (j, tid) in enumerate(batch):
                        first = (done == 0)
                        last = (done == ncl - 1)
                        done += 1
                        P.op('pe', lambda e, jj=jj, j=j: e.matmul(out=self.pb[bo][:, 0:128], lhsT=V[hp][:, j, :], rhs=p_t[:, jj, :], start=first, stop=last),
                             reads=[kv, kpt], writes=['pb%d' % bo])
                        P.op('pe', lambda e, jj=jj, j=j: e.matmul(out=self.pb[bd][:, 0:128], lhsT=self.ones_bf[:], rhs=p_t[:, jj, :], start=first, stop=last),
                             reads=['ones_bf', kpt], writes=['pb%d' % bd])
                P.op('dve', lambda e: e.reciprocal(out=rd[:], in_=self.pb[bd][:, 0:128]), reads=['pb%d' % bd], writes=[krd])
                P.op('dve', lambda e: e.tensor_tensor(out=self.ybT[:, h, i * 128:(i + 1) * 128], in0=self.pb[bo][:, 0:128], in1=rd[:], op=ALU.mult),
                     reads=['pb%d' % bo, krd], writes=['ybT'])
        P.pop()

    def outproj_stage(self, layer, src, wslots):
        P = self.P
        P.push()
        xb = [P.sb("oxb%d" % i, [128, D], F32) for i in range(2)]
        xo = [P.sb("oxo%d" % i, [128, D], F32) for i in range(2)]
        yt = [P.sb("oyt%d" % i, [128, 8, 128], BF16) for i in range(2)]
        self.pb_n = 0
        for tt in range(NT):
            p2 = tt % 2
            P.dma('sp', xb[p2][:], src[tt * 128:(tt + 1) * 128, :], reads=['xs'], writes=['oxb%d' % p2])
            if layer == 0:
                P.dma('sp', yt[p2][:], self.yTd[tt], reads=['yTd'], writes=['oyt%d' % p2])
            for db in range(4):
                bi = self.bank()
                for kc in range(KC):
                    if kc < 8:
                        if layer == 0:
                            lhsT, kl = yt[p2][:, kc, :], 'oyt%d' % p2
                        else:
                            lhsT, kl = self.yaT[:, kc, tt * 128:(tt + 1) * 128], 'yaT'
                    else:
                        lhsT, kl = self.ybT[:, kc - 8, tt * 128:(tt + 1) * 128], 'ybT'
                    P.op('pe', lambda e, kc=kc, lhsT=lhsT: e.matmul(out=self.pb[bi][:], lhsT=lhsT, rhs=self.ring[wslots[db]][:, kc, :],
                                                                   start=(kc == 0), stop=(kc == KC - 1)),
                         reads=[kl, 'ring%d' % wslots[db]], writes=['pb%d' % bi])
                P.op('dve', lambda e: e.tensor_tensor(out=xo[p2][:, db * 512:(db + 1) * 512], in0=self.pb[bi][:], in1=xb[p2][:, db * 512:(db + 1) * 512], op=ALU.add),
                     reads=['pb%d' % bi, 'oxb%d' % p2], writes=['oxo%d' % p2])
            P.dma('sp', self.xs[tt * 128:(tt + 1) * 128, :], xo[p2][:], reads=['oxo%d' % p2], writes=['xs_w'])
        P.pop()

    def conv_stage(self, w_in, pre):
        P = self.P
        P.push()
        taps = P.sb("taps", [128, 8, 3], F32)
        P.dma('sp', taps[:], self.taps, writes=['taps'])
        z = P.sb("cz", [128, S + 2], F32)
        yv = P.sb("cyv", [128, S], F32)
        csb = [P.sb("ccsb%d" % i, [128, 512], F32) for i in range(2)]
        P.op('dve', lambda e: e.memset(z[:], 0.0), writes=['cz'])
        n = 0
        for hf in range(2):
            if hf == 0:
                slab_c, slab_x, slab_b = pre
            else:
                slab_c = self.load_slab(w_in, 1024 + hf * 512)
                slab_x = self.load_slab(w_in, 2048 + hf * 512)
                slab_b = self.load_slab(w_in, hf * 512)
            for cc4 in range(4):
                cc = hf * 4 + cc4
                for tb in range(4):
                    bc = self.bank()
                    bx = self.bank()
                    for (bi, slab) in ((bc, slab_c), (bx, slab_x)):
                        for kc in range(KC):
                            P.op('pe', lambda e, kc=kc, bi=bi, slab=slab: e.matmul(out=self.pb[bi][:], lhsT=self.ring[slab][:, kc, cc4 * 128:(cc4 + 1) * 128],
                                                                                  rhs=self.hT[:, kc, tb * 512:(tb + 1) * 512], start=(kc == 0), stop=(kc == KC - 1)),
                                 reads=['hT', 'ring%d' % slab], writes=['pb%d' % bi])
                    cs = csb[n % 2]
                    kcs = 'ccsb%d' % (n % 2)
                    n += 1
                    P.op('act', lambda e: e.copy(out=cs[:], in_=self.pb[bc][:]), reads=['pb%d' % bc], writes=[kcs])
                    P.op('dve', lambda e: e.tensor_tensor(out=z[:, 1 + tb * 512:1 + (tb + 1) * 512], in0=cs[:], in1=self.pb[bx][:], op=ALU.mult),
                         reads=[kcs, 'pb%d' % bx], writes=['cz'])
                P.op('dve', lambda e: e.tensor_scalar(out=yv[:], in0=z[:, 0:S], scalar1=taps[:, cc, 0:1], scalar2=None, op0=ALU.mult),
                     reads=['cz', 'taps'], writes=['cyv'])
                P.op('dve', lambda e: e.scalar_tensor_tensor(out=yv[:], in0=z[:, 1:S + 1], scalar=taps[:, cc, 1:2], in1=yv[:], op0=ALU.mult, op1=ALU.add),
                     reads=['cz', 'taps', 'cyv'], writes=['cyv'])
                P.op('dve', lambda e: e.scalar_tensor_tensor(out=yv[:], in0=z[:, 2:S + 2], scalar=taps[:, cc, 2:3], in1=yv[:], op0=ALU.mult, op1=ALU.add),
                     reads=['cz', 'taps', 'cyv'], writes=['cyv'])
                for tb in range(4):
                    bb = self.bank()
                    for kc in range(KC):
                        P.op('pe', lambda e, kc=kc: e.matmul(out=self.pb[bb][:], lhsT=self.ring[slab_b][:, kc, cc4 * 128:(cc4 + 1) * 128],
                                                            rhs=self.hT[:, kc, tb * 512:(tb + 1) * 512], start=(kc == 0), stop=(kc == KC - 1)),
                             reads=['hT', 'ring%d' % slab_b], writes=['pb%d' % bb])
                    P.op('dve', lambda e: e.tensor_tensor(out=self.yaT[:, cc, tb * 512:(tb + 1) * 512], in0=self.pb[bb][:], in1=yv[:, tb * 512:(tb + 1) * 512], op=ALU.mult),
                         reads=['pb%d' % bb, 'cyv'], writes=['yaT'])
        P.pop()

    def moe(self, layer):
        P = self.P
        P.push()
        self.aff = P.sb("aff", [128, NT, NEXP], F32)
        mask_tm = P.sb("mask_tm", [128, NT, NEXP], F32)
        pos_tm = P.sb("pos_tm", [128, NT, NEXP], F32)
        rg = P.sb("rg", [128, NT, NEXP, 2], F32)
        iota = P.sb("iota", [128, 256], F32)
        tokidx = P.sb("tokidx", [128, NT], F32)
        P.dma('sp', iota[:], self.iota_in, writes=['iota'])
        P.dma('sp', tokidx[:], self.tokidx_in, writes=['tokidx'])
        wg, wu, wd = self.w_gate[layer], self.w_up[layer], self.w_down[layer]
        specs = []
        for ex_ in range(NEXP):
            for fs in range(4):
                specs.append((wg[ex_], fs * 512))
                specs.append((wu[ex_], fs * 512))
            for ds_ in range(4):
                specs.append((wd[ex_], ds_ * 512))
        sched = {'issued': 0, 'taken': 0, 'slots': []}

        def issue():
            n = sched['issued']
            if n < len(specs):
                sched['slots'].append(self.load_slab(*specs[n]))
                sched['issued'] = n + 1

        def take():
            sl = sched['slots'][sched['taken']]
            sched['taken'] += 1
            return sl

        for _ in range(4):
            issue()
        self.norm_stage(self.xs, self.norm_ffn[layer], 'moe', layer)
        P.push()
        affT = P.sb("affT", [NEXP, S], F32)
        work = P.sb("rwork", [NEXP, S], F32)
        onesT = P.sb("ronesT", [NEXP, S], F32)
        m8 = P.sb("rm8", [NEXP, 8], F32)
        for b4 in range(4):
            bi = self.bank()
            for j in range(4):
                tt = b4 * 4 + j
                P.op('pe', lambda e, tt=tt, j=j: e.transpose(out=self.pb[bi][0:NEXP, j * 128:(j + 1) * 128], in_=self.aff[:, tt, :], identity=self.ident[:]),
                     reads=['aff', 'ident'], writes=['pb%d' % bi])
            P.op('dve', lambda e: e.tensor_copy(out=affT[:, b4 * 512:(b4 + 1) * 512], in_=self.pb[bi][0:NEXP, :]), reads=['pb%d' % bi], writes=['affT'])
        P.op('dve', lambda e: e.tensor_copy(out=work[:], in_=affT[:]), reads=['affT'], writes=['rwork'])
        P.op('dve', lambda e: e.memset(onesT[:], 1.0), writes=['ronesT'])
        for r in range(CAP // 8):
            P.op('dve', lambda e: e.max(out=m8[:], in_=work[:]), reads=['rwork'], writes=['rm8'])
            if r < CAP // 8 - 1:
                P.op('dve', lambda e: e.match_replace(out=work[:], in_to_replace=m8[:], in_values=work[:], imm_value=-1.0),
                     reads=['rm8', 'rwork'], writes=['rwork'])
        P.op('dve', lambda e: e.tensor_scalar(out=work[:], in0=affT[:], scalar1=m8[:, 7:8], scalar2=None, op0=ALU.is_ge),
             reads=['affT', 'rm8', 'rwork'], writes=['rwork'])
        P.op('dve', lambda e: e.tensor_tensor_scan(out=affT[:], data0=onesT[:], data1=work[:], initial=0.0, op0=ALU.mult, op1=ALU.add),
             reads=['ronesT', 'rwork', 'affT'], writes=['affT'])
        P.op('dve', lambda e: e.tensor_tensor(out=affT[:], in0=affT[:], in1=work[:], op=ALU.subtract), reads=['affT', 'rwork'], writes=['affT'])
        for (srcT, ksrc, dst, kdst) in ((work, 'rwork', mask_tm, 'mask_tm'), (affT, 'affT', pos_tm, 'pos_tm')):
            bi = self.bank()
            for tt in range(NT):
                P.op('pe', lambda e, tt=tt: e.transpose(out=self.pb[bi][:, tt * NEXP:(tt + 1) * NEXP], in_=srcT[:, tt * 128:(tt + 1) * 128],
                                                       identity=self.ident[0:NEXP, 0:NEXP]), reads=[ksrc, 'ident'], writes=['pb%d' % bi])
            P.op('dve', lambda e: e.tensor_copy(out=dst[:].rearrange("p t e -> p (t e)"), in_=self.pb[bi][:, 0:NT * NEXP]), reads=['pb%d' % bi], writes=[kdst])
        P.pop()
        P.op('dve', lambda e: e.tensor_copy(out=rg[:, :, :, 0], in_=tokidx[:, :].unsqueeze(2).to_broadcast([128, NT, NEXP])), reads=['tokidx'], writes=['rg'])
        P.op('dve', lambda e: e.tensor_copy(out=rg[:, :, :, 1], in_=self.aff[:]), reads=['aff', 'rg'], writes=['rg'])
        Sel = [P.sb("Sel%d" % i, [128, NT, CAP], F32) for i in range(2)]
        xe = [P.sb("xe%d" % i, [128, 2, D], F32) for i in range(2)]
        ye = [P.sb("ye%d" % i, [128, 2, D], F32) for i in range(2)]
        ig = [P.sb("ig%d" % i, [128, 4], F32) for i in range(2)]
        idx = [P.sb("idx%d" % i, [128, 2], I32) for i in range(2)]
        xeT = P.sb("xeT", [128, KC, CAP], BF16)
        hidT = P.sb("hidT", [128, KC, CAP], BF16)
        sg = [P.sb("sg%d" % i, [128, CAP], F32) for i in range(2)]

        def prep(e_):
            p2 = e_ % 2
            kS, kig, kidx, kxe = 'Sel%d' % p2, 'ig%d' % p2, 'idx%d' % p2, 'xe%d' % p2
            for tt in range(NT):
                P.op('dve', lambda e, tt=tt: e.tensor_scalar(out=Sel[p2][:, tt, :], in0=iota[:], scalar1=pos_tm[:, tt, e_:e_ + 1],
                                                            scalar2=mask_tm[:, tt, e_:e_ + 1], op0=ALU.is_equal, op1=ALU.mult),
                     reads=['iota', 'pos_tm', 'mask_tm'], writes=[kS])
            bi = self.bank()
            for jc in range(2):
                for tt in range(NT):
                    P.op('pe', lambda e, tt=tt, jc=jc: e.matmul(out=self.pb[bi][:, jc * 2:jc * 2 + 2], lhsT=Sel[p2][:, tt, jc * 128:(jc + 1) * 128],
                                                               rhs=rg[:, tt, e_, :], start=(tt == 0), stop=(tt == NT - 1)),
                         reads=[kS, 'rg'], writes=['pb%d' % bi])
            P.op('dve', lambda e: e.tensor_copy(out=ig[p2][:], in_=self.pb[bi][:, 0:4]), reads=['pb%d' % bi], writes=[kig])
            P.op('dve', lambda e: e.tensor_copy(out=idx[p2][:], in_=ig[p2][:, 0:4:2]), reads=[kig], writes=[kidx])
            for jc in range(2):
                P.dma('pool', lambda g, jc=jc: g.indirect_dma_start(out=xe[p2][:, jc, :], out_offset=None, in_=self.h2d,
                                                                   in_offset=bass.IndirectOffsetOnAxis(ap=idx[p2][:, jc:jc + 1], axis=0)),
                      reads=[kidx, 'h2d'], writes=[kxe])

        prep(0)
        for ex_ in range(NEXP):
            p2 = ex_ % 2
            kig, kidx, kxe, kye = 'ig%d' % p2, 'idx%d' % p2, 'xe%d' % p2, 'ye%d' % p2
            for k2 in range(KC // 2):
                bi = self.bank()
                for q in range(4):
                    kc = k2 * 2 + q // 2
                    jc = q % 2
                    P.op('pe', lambda e, kc=kc, jc=jc, q=q: e.transpose(out=self.pb[bi][:, q * 128:(q + 1) * 128], in_=xe[p2][:, jc, kc * 128:(kc + 1) * 128],
                                                                       identity=self.ident[:]), reads=[kxe, 'ident'], writes=['pb%d' % bi])
                dst = xeT[:, k2 * 2:k2 * 2 + 2, :].rearrange("p a b -> p (a b)")
                if k2 % 2 == 0:
                    P.op('act', lambda e: e.copy(out=dst, in_=self.pb[bi][:]), reads=['pb%d' % bi], writes=['xeT'])
                else:
                    P.op('dve', lambda e: e.tensor_copy(out=dst, in_=self.pb[bi][:]), reads=['pb%d' % bi], writes=['xeT'])
            for fs in range(4):
                sl_g = take()
                sl_u = take()
                for fc in range(4):
                    bi = self.bank()
                    for (off, slot) in ((0, sl_g), (256, sl_u)):
                        for kc in range(KC):
                            P.op('pe', lambda e, kc=kc, off=off, slot=slot: e.matmul(out=self.pb[bi][:, off:off + 256], lhsT=self.ring[slot][:, kc, fc * 128:(fc + 1) * 128],
                                                                                    rhs=xeT[:, kc, :], start=(kc == 0), stop=(kc == KC - 1)),
                                 reads=['xeT', 'ring%d' % slot], writes=['pb%d' % bi])
                    s_ = sg[(fs * 4 + fc) % 2]
                    ks = 'sg%d' % ((fs * 4 + fc) % 2)
                    P.op('act', lambda e: e.activation(out=s_[:], in_=self.pb[bi][:, 0:256], func=AF.Silu), reads=['pb%d' % bi], writes=[ks])
                    P.op('dve', lambda e: e.tensor_tensor(out=hidT[:, fs * 4 + fc, :], in0=s_[:], in1=self.pb[bi][:, 256:512], op=ALU.mult),
                         reads=[ks, 'pb%d' % bi], writes=['hidT'])
                issue()
                issue()
            if ex_ + 1 < NEXP:
                prep(ex_ + 1)
            for ds_ in range(4):
                sl_d = take()
                for jc in range(2):
                    bi = self.bank()
                    for kc in range(KC):
                        P.op('pe', lambda e, kc=kc: e.matmul(out=self.pb[bi][:], lhsT=hidT[:, kc, jc * 128:(jc + 1) * 128], rhs=self.ring[sl_d][:, kc, :],
                                                            start=(kc == 0), stop=(kc == KC - 1)),
                             reads=['hidT', 'ring%d' % sl_d], writes=['pb%d' % bi])
                    dsty = ye[p2][:, jc, ds_ * 512:(ds_ + 1) * 512]
                    gsc = ig[p2][:, 2 * jc + 1:2 * jc + 2]
                    if jc == 0:
                        P.op('act', lambda e: e.activation(out=dsty, in_=self.pb[bi][:], func=AF.Copy, scale=gsc), reads=['pb%d' % bi, kig], writes=[kye])
                    else:
                        P.op('dve', lambda e: e.tensor_scalar(out=dsty, in0=self.pb[bi][:], scalar1=gsc, scalar2=None, op0=ALU.mult),
                             reads=['pb%d' % bi, kig], writes=[kye])
                issue()
            for jc in range(2):
                P.dma('pool', lambda g, jc=jc: g.indirect_dma_start(out=self.xs, out_offset=bass.IndirectOffsetOnAxis(ap=idx[p2][:, jc:jc + 1], axis=0),
                                                                   in_=ye[p2][:, jc, :], in_offset=None, compute_op=ALU.add),
                      reads=[kye, kidx, 'xs_w'], writes=['xs'])
        P.pop()


def _prep_inputs(inputs):
    f = lambda a: np.ascontiguousarray(np.asarray(a, dtype=np.float32))
    shared = {
        'norm_mix': f(inputs['norm_mix']), 'norm_ffn': f(inputs['norm_ffn']), 'norm_final': f(inputs['norm_final']),
        'w_in_ab': f(inputs['w_in_ab'][0]), 'a_v_norm': f(inputs['a_v_norm'][0]),
        'a_spatial_wT': f(np.transpose(np.asarray(inputs['a_spatial_w'][0]), (2, 0, 1))),
        'a_spatial_bT': f(np.transpose(np.asarray(inputs['a_spatial_b'][0]), (1, 0))),
        'na_tab': na_bias_table(np.asarray(inputs['b_rpb'][0], dtype=np.float32)),
        'w_out_ab': f(inputs['w_out_ab'][0]), 'w_in_cd': f(inputs['w_in_cd'][0]),
        'c_convT': f(np.transpose(np.asarray(inputs['c_conv'][0]).reshape(3, 8, 128), (2, 1, 0))),
        'dil_tab': dil_bias_table(),
        'w_out_cd': f(inputs['w_out_cd'][0]), 'router': f(inputs['router']),
        'w_gate': f(inputs['w_gate']), 'w_up': f(inputs['w_up']), 'w_down': f(inputs['w_down']),
        'ident': np.eye(128, dtype=np.float32),
        'iota': np.tile(np.arange(256, dtype=np.float32)[None, :], (128, 1)),
        'tokidx': f(np.arange(128)[:, None] + 128 * np.arange(NT)[None, :]),
    }
    return shared


def run(inputs, stop=None, trace=False, ncores=8, start=0):
    mk = MK(stop=stop, start=start)
    shared = _prep_inputs(inputs)
    x = np.asarray(inputs['x'], dtype=np.float32)
    in_maps = []
    for c in range(ncores):
        m = {k: v for k, v in shared.items() if k in mk.declared}
        m['x'] = np.ascontiguousarray(x[c])
        in_maps.append(m)
    res = run_bass_kernel_spmd(mk.nc, in_maps, core_ids=list(range(ncores)), trace=trace)
    out = np.stack([res.results[c]['out'] for c in range(ncores)], axis=0)
    return out, res


def kernel(**inputs):
    out, _ = run(inputs)
    return out.astype(np.float32)
```
